# Optimizing a Trainium2 kernel written in Bass

```python
import jax, jax.numpy as jnp
from jax import lax
import numpy as np

D_MODEL = 1024
BATCH = 8
SEQ = 4096
DEPTH = 1

HEAD_DIM = 64
A_HEADS = 8
A_KV_HEADS = 2
IDX_HEADS = 8
IDX_DIM = 64
TOPK_MAX = 256
B_HEADS = 8
B_KV_HEADS = 2
WINDOW = 128
Q_BLOCK = 128
D_FF = 4 * D_MODEL
ROPE_THETA = 10000.0
RMS_EPS = 1e-6
LN_EPS = 1e-6

A_Q = A_HEADS * HEAD_DIM
A_KV = A_KV_HEADS * HEAD_DIM
IDX_Q = IDX_HEADS * IDX_DIM
B_Q = B_HEADS * HEAD_DIM
B_KV = B_KV_HEADS * HEAD_DIM
SPLIT_SIZES = (A_Q, A_KV, A_KV, IDX_Q, IDX_DIM, IDX_HEADS, B_Q, B_KV, B_KV, D_MODEL, D_MODEL)
D_IN = sum(SPLIT_SIZES)

kernel_name = "hybrid_dsa_swa_sink_gated_block"


def rmsnorm(x, g):
    xf = x.astype(jnp.float32)
    y = xf * lax.rsqrt(jnp.mean(xf * xf, axis=-1, keepdims=True) + RMS_EPS)
    return (y * g.astype(jnp.float32)).astype(x.dtype)


def layernorm(x, w, b):
    xf = x.astype(jnp.float32)
    mu = jnp.mean(xf, axis=-1, keepdims=True)
    var = jnp.mean(jnp.square(xf - mu), axis=-1, keepdims=True)
    y = (xf - mu) * lax.rsqrt(var + LN_EPS)
    return (y * w.astype(jnp.float32) + b.astype(jnp.float32)).astype(x.dtype)


def rope_tables(L, dim):
    inv = 1.0 / (ROPE_THETA ** (jnp.arange(0, dim, 2, dtype=jnp.float32) / dim))
    ang = jnp.arange(L, dtype=jnp.float32)[:, None] * inv[None, :]
    return jnp.cos(ang), jnp.sin(ang)


def apply_rope(x, cos, sin):
    xf = x.astype(jnp.float32)
    x1, x2 = jnp.split(xf, 2, axis=-1)
    c = cos[None, :, None, :]
    s = sin[None, :, None, :]
    return jnp.concatenate([x1 * c - x2 * s, x2 * c + x1 * s], axis=-1).astype(x.dtype)


def dsa_branch(q, k, v, q_idx, k_idx, w_idx, topk):
    Bn, L, H, dh = q.shape
    G = k.shape[2]
    R = H // G
    nb = L // Q_BLOCK
    kpos = jnp.arange(L)
    k_idx32 = k_idx.astype(jnp.float32)
    scale = HEAD_DIM ** -0.5

    def one_block(i):
        start = i * Q_BLOCK
        qb = lax.dynamic_slice_in_dim(q, start, Q_BLOCK, axis=1)
        qib = lax.dynamic_slice_in_dim(q_idx, start, Q_BLOCK, axis=1)
        wb = lax.dynamic_slice_in_dim(w_idx, start, Q_BLOCK, axis=1)
        qpos = start + jnp.arange(Q_BLOCK)
        s = jnp.einsum('bqhd,bkd->bqhk', qib.astype(jnp.float32), k_idx32)
        score = jnp.einsum('bqhk,bqh->bqk', jax.nn.relu(s), wb.astype(jnp.float32))
        causal = kpos[None, :] <= qpos[:, None]
        score = jnp.where(causal[None], score, -jnp.inf)
        _, sel = lax.top_k(score, topk)
        ks = jax.vmap(lambda kk, ii: kk[ii])(k, sel)
        vs = jax.vmap(lambda vv, ii: vv[ii])(v, sel)
        valid = sel <= qpos[None, :, None]
        qg = qb.reshape(Bn, Q_BLOCK, G, R, dh).astype(jnp.float32)
        logits = jnp.einsum('bqgrd,bqkgd->bqgrk', qg, ks.astype(jnp.float32)) * scale
        logits = jnp.where(valid[:, :, None, None, :], logits, -jnp.inf)
        p = jax.nn.softmax(logits, axis=-1)
        o = jnp.einsum('bqgrk,bqkgd->bqgrd', p, vs.astype(jnp.float32))
        return o.reshape(Bn, Q_BLOCK, H * dh).astype(q.dtype)

    out = lax.map(one_block, jnp.arange(nb))
    return jnp.transpose(out, (1, 0, 2, 3)).reshape(Bn, L, H * dh)


def swa_branch(q, k, v, sinks):
    Bn, L, H, dh = q.shape
    G = k.shape[2]
    R = H // G
    W = WINDOW
    nb = L // W
    qb = q.reshape(Bn, nb, W, G, R, dh).astype(jnp.float32)
    pad = ((0, 0), (W, 0), (0, 0), (0, 0))
    kp = jnp.pad(k, pad).reshape(Bn, nb + 1, W, G, dh)
    vp = jnp.pad(v, pad).reshape(Bn, nb + 1, W, G, dh)
    kb = jnp.concatenate([kp[:, :-1], kp[:, 1:]], axis=2).astype(jnp.float32)
    vb = jnp.concatenate([vp[:, :-1], vp[:, 1:]], axis=2).astype(jnp.float32)
    logits = jnp.einsum('bnqgrd,bnkgd->bngrqk', qb, kb) * (HEAD_DIM ** -0.5)
    rel = jnp.arange(W)[:, None] + W - jnp.arange(2 * W)[None, :]
    kabs = jnp.arange(nb)[:, None] * W - W + jnp.arange(2 * W)[None, :]
    mask = ((rel >= 0) & (rel < W))[None, :, :] & (kabs >= 0)[:, None, :]
    logits = jnp.where(mask[None, :, None, None, :, :], logits, -jnp.inf)
    sink = jnp.broadcast_to(sinks.astype(jnp.float32).reshape(1, 1, G, R, 1, 1), logits.shape[:-1] + (1,))
    p = jax.nn.softmax(jnp.concatenate([logits, sink], axis=-1), axis=-1)[..., :-1]
    o = jnp.einsum('bngrqk,bnkgd->bnqgrd', p, vb)
    return o.reshape(Bn, L, H * dh).astype(q.dtype)


def setup_inputs(seed: int = 0) -> dict:
    key = jax.random.key(seed)
    ks = jax.random.split(key, 14)
    f32 = jnp.float32
    nrm = lambda k, shape: jax.random.normal(k, shape, dtype=f32)
    return {
        "x": nrm(ks[0], (BATCH, SEQ, D_MODEL)),
        "attn_norm_w": 1.0 + 0.02 * nrm(ks[1], (DEPTH, D_MODEL)),
        "w_in": nrm(ks[2], (DEPTH, D_MODEL, D_IN)) * D_MODEL ** -0.5,
        "idx_k_norm_w": 1.0 + 0.02 * nrm(ks[3], (DEPTH, IDX_DIM)),
        "idx_k_norm_b": 0.02 * nrm(ks[4], (DEPTH, IDX_DIM)),
        "sinks": 0.5 * nrm(ks[5], (DEPTH, B_HEADS)),
        "w_branch_a": nrm(ks[6], (DEPTH, A_Q, D_MODEL)) * A_Q ** -0.5,
        "w_branch_b": nrm(ks[7], (DEPTH, B_Q, D_MODEL)) * B_Q ** -0.5,
        "w_out": nrm(ks[8], (DEPTH, D_MODEL, D_MODEL)) * D_MODEL ** -0.5,
        "mlp_norm_w": 1.0 + 0.02 * nrm(ks[9], (DEPTH, D_MODEL)),
        "w_up": nrm(ks[10], (DEPTH, D_MODEL, D_FF)) * D_MODEL ** -0.5,
        "w_down": nrm(ks[11], (DEPTH, D_FF, D_MODEL)) * D_FF ** -0.5,
        "final_norm_w": 1.0 + 0.02 * nrm(ks[12], (D_MODEL,)),
    }


def reference(x, attn_norm_w, w_in, idx_k_norm_w, idx_k_norm_b, sinks, w_branch_a, w_branch_b,
              w_out, mlp_norm_w, w_up, w_down, final_norm_w):
    Bn, L, _ = x.shape
    topk = min(TOPK_MAX, L // 4)
    cos, sin = rope_tables(L, HEAD_DIM)
    cos_i, sin_i = rope_tables(L, IDX_DIM)
    offsets = [sum(SPLIT_SIZES[:i + 1]) for i in range(len(SPLIT_SIZES) - 1)]
    w_scale = (IDX_HEADS ** -0.5) * (IDX_DIM ** -0.5)

    for l in range(DEPTH):
        h = rmsnorm(x, attn_norm_w[l])
        proj = jnp.einsum('bld,de->ble', h, w_in[l])
        a_q, a_k, a_v, i_q, i_k, i_w, b_q, b_k, b_v, g_a, g_b = jnp.split(proj, offsets, axis=-1)

        a_q = apply_rope(a_q.reshape(Bn, L, A_HEADS, HEAD_DIM), cos, sin)
        a_k = apply_rope(a_k.reshape(Bn, L, A_KV_HEADS, HEAD_DIM), cos, sin)
        a_v = a_v.reshape(Bn, L, A_KV_HEADS, HEAD_DIM)
        i_q = apply_rope(i_q.reshape(Bn, L, IDX_HEADS, IDX_DIM), cos_i, sin_i)
        i_k = layernorm(i_k, idx_k_norm_w[l], idx_k_norm_b[l])
        i_k = apply_rope(i_k[:, :, None, :], cos_i, sin_i)[:, :, 0, :]
        i_w = i_w * w_scale
        o_a = dsa_branch(a_q, a_k, a_v, i_q, i_k, i_w, topk)

        b_q = apply_rope(b_q.reshape(Bn, L, B_HEADS, HEAD_DIM), cos, sin)
        b_k = apply_rope(b_k.reshape(Bn, L, B_KV_HEADS, HEAD_DIM), cos, sin)
        b_v = b_v.reshape(Bn, L, B_KV_HEADS, HEAD_DIM)
        o_b = swa_branch(b_q, b_k, b_v, sinks[l])

        y_a = jnp.einsum('ble,ed->bld', o_a, w_branch_a[l])
        y_b = jnp.einsum('ble,ed->bld', o_b, w_branch_b[l])
        mix = jax.nn.sigmoid(g_a) * y_a + jax.nn.sigmoid(g_b) * y_b
        x = x + jnp.einsum('bld,de->ble', mix, w_out[l])

        h2 = rmsnorm(x, mlp_norm_w[l])
        u = jax.nn.relu(jnp.einsum('bld,df->blf', h2, w_up[l]))
        x = x + jnp.einsum('blf,fd->bld', u * u, w_down[l])

    return rmsnorm(x, final_norm_w)
```

```python
import numpy as np
import ml_dtypes
from contextlib import ExitStack
import concourse.bass as bass
import concourse.mybir as mybir
from concourse.bass_utils import run_bass_kernel_spmd

F32 = mybir.dt.float32
BF16 = mybir.dt.bfloat16
ALU = mybir.AluOpType
AF = mybir.ActivationFunctionType
AX = mybir.AxisListType

D = 1024
DIN = 4168
DFF = 4096
TOPK = 256
NEG = -30000.0
SNEG = -1.0e30
W_SCALE = (8 ** -0.5) * (64 ** -0.5)
NIT = 18
NDMA = 12
RMS_EPS = 1e-6
LN_EPS = 1e-6

C_AQ, C_IQ, C_BQ, C_AK, C_BK = 0, 512, 1024, 1536, 1664
C_GA, C_GB = 1792, 2816
C_TM = 3840
N_TM = 328


class _Stop(Exception):
    pass


STOP_AFTER = None


def _chk(stage):
    if STOP_AFTER == stage:
        raise _Stop()


class Sched:
    ENG = ("sp", "pe", "act", "dve", "pool")

    def __init__(self):
        self.ops = {e: [] for e in self.ENG}
        self.count = {e: 0 for e in self.ENG}
        self.waited = {e: {} for e in self.ENG}
        self.buf_w = {}
        self.buf_r = {}
        self.dma_val = [0] * NDMA
        self.next_dma = 0

    def add(self, eng, fns, reads=(), writes=(), dma=False):
        if not isinstance(fns, (list, tuple)):
            fns = [fns]
        writes = list(writes) + [b for b in reads if isinstance(b, tuple) and b[0] == "ps" and b not in writes]
        deps = {}

        def need(k, v, kind):
            if k == eng and eng == "pe":
                return
            if deps.get(k, 0) < v:
                deps[k] = v

        for b in reads:
            w = self.buf_w.get(b)
            if w:
                need(w[0], w[1], "raw")
        for b in writes:
            w = self.buf_w.get(b)
            if w:
                need(w[0], w[1], "waw")
            for k, v in self.buf_r.get(b, {}).items():
                need(k, v, "war")
        if dma:
            s = self.next_dma
            self.next_dma = (s + 1) % NDMA
            if self.dma_val[s] > 0:
                need(("dma", s), self.dma_val[s], "raw")
            self.dma_val[s] += 16
            me = (("dma", s), self.dma_val[s])
        else:
            self.count[eng] += 1
            me = (eng, self.count[eng])
        waits = []
        for k, v in deps.items():
            if self.waited[eng].get(k, 0) >= v:
                continue
            self.waited[eng][k] = v
            waits.append((k, v))
        self.ops[eng].append((list(fns), waits, me, dma))
        for b in reads:
            d = self.buf_r.setdefault(b, {})
            if d.get(me[0], 0) < me[1]:
                d[me[0]] = me[1]
        for b in writes:
            self.buf_w[b] = me
            self.buf_r[b] = {}
        return me


def build_program(L, dbg=()):
    NT, NG = L // 128, L // 512
    nc = bass.Bass("TRN2", target_bir_lowering=False)

    def din(name, shape, dt=F32):
        return nc.dram_tensor(name, list(shape), dt, kind="ExternalInput").ap()

    x_d = din("x", [L, D])
    win_d = din("w_in", [D, DIN])
    wba_d = din("w_ba", [512, D])
    wbb_d = din("w_bb", [512, D])
    wout_d = din("w_out", [D, D])
    wup_d = din("w_up", [D, DFF])
    wdn_d = din("w_dn", [DFF, D])
    anw_d = din("anw", [128, 8])
    mnw_d = din("mnw", [128, 8])
    fnw_d = din("fnw", [128, D])
    iknw_d = din("iknw", [128, 64])
    iknb_d = din("iknb", [128, 64])
    sinks_d = din("sinks", [128, 8])
    cf_d = din("ropeCf", [128, L])
    sf_d = din("ropeSf", [128, L])
    ct_d = din("ropeCt", [128, NT * 32])
    st_d = din("ropeSt", [128, NT * 32])
    ident_d = din("ident", [128, 128], BF16)
    ident4_d = din("ident4", [128, 512], BF16)
    rotT_d = din("rotT", [128, 128], BF16)
    swac_d = din("swa_cur", [128, 512], BF16)
    swap_d = din("swa_prev", [128, 512], BF16)
    cmask_d = din("cmask", [128, 128], BF16)
    pow2_d = din("pow2", [128, NIT + 1])
    out_d = nc.dram_tensor("out", [L, D], F32, kind="ExternalOutput").ap()
    dbg_d = {}

    def dscr(name, shape):
        return nc.dram_tensor(name, list(shape), BF16, kind="Internal").ap()

    s_in = dscr("s_in", [128, 8, DIN])
    s_ba = dscr("s_ba", [128, 4, D])
    s_bb = dscr("s_bb", [128, 4, D])
    s_out = dscr("s_out", [128, 8, D])
    s_up = dscr("s_up", [128, 8, DFF])
    s_dn = dscr("s_dn", [128, 32, D])

    S = Sched()
    es = ExitStack()

    def sb(name, shape, dt):
        return es.enter_context(nc.sbuf_tensor("sb_" + name, list(shape), dt))

    LS = max(L, 4096)
    akT = sb("akT", [128, L], BF16)
    ikT = sb("ikT", [128, L], BF16)
    av = sb("av", [128, NT * 130], BF16)
    bkT = sb("bkT", [128, 640], BF16)
    bv = sb("bv", [128, 5 * 130], BF16)
    xg = sb("xg", [128, 4096], F32)
    hT = sb("hT", [128, 4096], BF16)
    oT = sb("oT", [128, 4096], BF16)
    qall = sb("qall", [128, 24 * 512], BF16)
    iw = sb("iw", [128, 32], F32)
    scs = [sb(f"scores{i}", [128, LS], F32) for i in range(2)]
    scores = scs[0]
    mbs = [sb(f"mb{i}", [128, L], BF16) for i in range(2)]
    Rb = [sb(f"R{i}", [128, 512], BF16) for i in range(3)]
    PTb = [sb(f"PT{i}", [128, 512], BF16) for i in range(3)]
    diags = [sb(f"diag{i}", [128, 1024], BF16) for i in range(2)]
    wsl = [sb(f"wsl{i}", [128, 4096], BF16) for i in range(4)]
    cfg = sb("cfg", [128, 512], F32)
    sfg = sb("sfg", [128, 512], F32)
    ctT = sb("ctT", [128, 4 * 32], F32)
    stT = sb("stT", [128, 4 * 32], F32)
    fnw = sb("fnw", [128, D], F32)
    iknw = sb("iknw", [128, 64], F32)
    iknb = sb("iknb", [128, 64], F32)
    esink = sb("esink", [128, 8], F32)
    anw = sb("anw", [128, 8], F32)
    mnw = sb("mnw", [128, 8], F32)
    ident = sb("ident", [128, 128], BF16)
    ident4 = sb("ident4", [128, 512], BF16)
    rotT = sb("rotT", [128, 128], BF16)
    swac = sb("swac", [128, 512], BF16)
    swap = sb("swap", [128, 512], BF16)
    cmask = sb("cmask", [128, 128], BF16)
    pow2 = sb("pow2", [128, NIT + 1], F32)
    neghalf = sb("neghalf", [128, 1], F32)
    T1 = [sb(f"t1_{i}", [128, 512], F32) for i in range(2)]
    T2 = [sb(f"t2_{i}", [128, 512], F32) for i in range(2)]
    pbb = [sb(f"pb{i}", [128, 512], BF16) for i in range(2)]
    xn = [sb(f"xn{i}", [128, D], BF16) for i in range(1)]
    onat = [sb(f"onat{i}", [128, 512], BF16) for i in range(3)]
    obuf = [scs[0][:, 0:D]]
    OBK = ("scores", 0)
    junk = wsl[0][:].bitcast(mybir.dt.uint8)
    JKK = ("w", 0)
    u2Tt = [sb(f"u2T{i}", [128, 2048], BF16) for i in range(1)] * 2
    sm = sb("sm", [128, 64], F32)
    hwk = sb("hwk", [128, NIT + 1], F32)
    ikx = sb("ikx", [128, 64], F32)
    ikc = sb("ikc", [128, 64], F32)
    ikr = sb("ikr", [128, 128], BF16)
    dtmp = sb("dtmp", [128, 128], F32)
    rd = sb("rd", [128, 8], F32)

    banks = [es.enter_context(nc.psum_tensor(f"ps{i}", [128, 512], F32)) for i in range(7)]
    ptb = es.enter_context(nc.psum_tensor("ptb", [128, 1024], BF16))
    PTK = ("ps", "pt")

    def B(i):
        return ("ps", i)

    sems = {}
    for e in Sched.ENG:
        sems[e] = es.enter_context(nc.semaphore(f"sem_{e}"))
    for i in range(NDMA):
        sems[("dma", i)] = es.enter_context(nc.semaphore(f"sem_dma{i}"))

    def v3(t, a, b):
        return t[:].rearrange("p (a b) -> p a b", a=a, b=b)

    xg3 = v3(xg, 4, D)
    hT3 = v3(hT, 8, 512)
    oT3 = v3(oT, 8, 512)
    q3 = v3(qall, 24, 512)
    mixT3 = scs[1][:].bitcast(BF16)[:, 0:4096].rearrange("p (a b) -> p a b", a=8, b=512)
    av4 = av[:].rearrange("p (t g e) -> p t g e", g=2, e=65)
    bv4 = bv[:].rearrange("p (t g e) -> p t g e", g=2, e=65)
    diag3s = [v3(d_, 8, 128) for d_ in diags]
    u2Ts = [v3(t_, 4, 512) for t_ in u2Tt]
    ptb3 = ptb[:].rearrange("p (a b) -> p a b", a=8, b=128)
    iw3 = v3(iw, 4, 8)
    ct3 = ctT[:].rearrange("p (t f) -> p t f", f=32)
    st3 = stT[:].rearrange("p (t f) -> p t f", f=32)

    def dma(out, in_, reads, writes):
        return S.add("sp", lambda e: e.dma_start(out=out, in_=in_), reads, writes, dma=True)

    def mm(out, lhsT, rhs, start, stop):
        return lambda e: e.matmul(out, lhsT, rhs, start=start, stop=stop, skip_group_check=True)

    def tr(out, in_):
        return lambda e: e.transpose(out, in_, ident[:])

    def act(out, in_, func, scale=1.0, accum_out=None):
        if accum_out is None:
            return lambda e: e.activation(out, in_, func, scale=scale)
        return lambda e: e.activation(out, in_, func, scale=scale, accum_out=accum_out)

    def ts(out, in0, s1, s2, op0, op1=None, accum_out=None):
        if op1 is None:
            return lambda e: e.tensor_scalar(out, in0, s1, None, op0)
        if accum_out is None:
            return lambda e: e.tensor_scalar(out, in0, s1, s2, op0, op1)
        return lambda e: e.tensor_scalar(out, in0, s1, s2, op0, op1, accum_out)

    def tt(out, in0, in1, op):
        return lambda e: e.tensor_tensor(out, in0, in1, op)

    def stt(out, in0, scalar, in1, op0, op1):
        return lambda e: e.scalar_tensor_tensor(out, in0, scalar, in1, op0, op1)

    def cp(out, in_):
        return lambda e: e.tensor_copy(out, in_)

    smi = [0]

    def smcol():
        i = smi[0] % 64
        smi[0] += 1
        return sm[:, i:i + 1], ("sm", i)

    def rsqrt_mean(ssq_ap, ssq_key, n, eps):
        a, ak = smcol()
        r, rk = smcol()
        S.add("dve", ts(a, ssq_ap, 1.0 / n, eps, ALU.mult, ALU.add), [ssq_key], [ak])
        S.add("pool", tt(r, a, neghalf[:], ALU.pow), [ak, "neghalf"], [rk])
        return r, rk

    def dump(name, ap, shape, dt, key):
        if name not in dbg:
            return
        d_ = nc.dram_tensor("dbg_" + name, list(shape), dt, kind="ExternalOutput").ap()
        dbg_d[name] = d_
        dma(d_, ap, [key] if not isinstance(key, list) else key, [])

    try:
        for t, d_, k in ((ident, ident_d, "ident"), (ident4, ident4_d, "ident4"), (rotT, rotT_d, "rotT"),
                         (swac, swac_d, "swac"), (swap, swap_d, "swap"), (cmask, cmask_d, "cmask"),
                         (pow2, pow2_d, "pow2"),
                         (fnw, fnw_d, "fnw"), (iknw, iknw_d, "iknw"), (iknb, iknb_d, "iknb"),
                         (esink, sinks_d, "esink"), (anw, anw_d, "anw"), (mnw, mnw_d, "mnw")):
            dma(t[:], d_, [], [k])
        S.add("dve", lambda e: e.memset(neghalf[:], -0.5), [], ["neghalf"])
        S.add("act", act(esink[:], esink[:], AF.Exp), ["esink"], ["esink"])
        S.add("pool", lambda e: e.memset(av[:], 1.0), [], ["av"])
        S.add("pool", lambda e: e.memset(bv[:], 1.0), [], ["bv"])
        S.add("pool", lambda e: e.memset(bkT[:], 0.0), [], ["bkT"])
        S.add("pool", lambda e: e.memset(qall[:], 0.0), [], [("q", i_) for i_ in range(24)])

        _chk("consts")
        stg = [xg, scores]
        stgk = ["xg_all", ("scores", 0)]
        cvo = [hT, oT]
        cvok = ["hT_all", "oT_all"]
        prep_i = [0]

        prep_list = []

        def prep_block(src3, dst3, nch, ncols, fold, scale=None):
            prep_list.append((src3, dst3, nch, ncols, fold, scale))

        def prep_in(i):
            src3, dst3, nch, ncols, fold, scale = prep_list[i]
            sv = stg[i % 2][:, 0:nch * ncols].rearrange("p (a b) -> p a b", a=nch, b=ncols)
            dma(sv, src3, [], [stgk[i % 2]])

        def prep_cv_out(i):
            src3, dst3, nch, ncols, fold, scale = prep_list[i]
            j = i % 2
            sv = stg[j][:, 0:nch * ncols].rearrange("p (a b) -> p a b", a=nch, b=ncols)
            ov = cvo[j][:, 0:nch * ncols].rearrange("p (a b) -> p a b", a=nch, b=ncols)
            if fold is not None:
                fns = [ts(ov[:, c, :], sv[:, c, :], fold[:, c:c + 1], None, ALU.mult) for c in range(nch)]
                S.add("dve", fns, [stgk[j], "anw", "mnw"], [cvok[j]])
            elif scale is not None:
                S.add("act", act(cvo[j][:, 0:nch * ncols], stg[j][:, 0:nch * ncols], AF.Identity, scale=scale),
                      [stgk[j]], [cvok[j]])
            else:
                S.add("act", act(cvo[j][:, 0:nch * ncols], stg[j][:, 0:nch * ncols], AF.Copy),
                      [stgk[j]], [cvok[j]])
            dma(dst3, ov, [cvok[j]], ["scr"])

        win3 = win_d.rearrange("(c p) n -> p c n", p=128)
        for c0 in range(0, DIN, 512):
            c1 = min(DIN, c0 + 512)
            prep_block(win3[:, :, c0:c1], s_in[:, :, c0:c1], 8, c1 - c0, anw)
        prep_block(wba_d.rearrange("(c p) n -> p c n", p=128), s_ba, 4, D, None)
        prep_block(wbb_d.rearrange("(c p) n -> p c n", p=128), s_bb, 4, D, None)
        wout3 = wout_d.rearrange("(c p) n -> p c n", p=128)
        for c0 in range(0, D, 512):
            prep_block(wout3[:, :, c0:c0 + 512], s_out[:, :, c0:c0 + 512], 8, 512, None, scale=0.5)
        wup3 = wup_d.rearrange("(c p) n -> p c n", p=128)
        for c0 in range(0, DFF, 512):
            prep_block(wup3[:, :, c0:c0 + 512], s_up[:, :, c0:c0 + 512], 8, 512, mnw)
        wdn3 = wdn_d.rearrange("(c p) n -> p c n", p=128)
        for c0 in range(0, 32, 4):
            prep_block(wdn3[:, c0:c0 + 4, :], s_dn[:, c0:c0 + 4, :], 4, D, None)
        prep_in(0)
        for i_ in range(len(prep_list)):
            if i_ + 1 < len(prep_list):
                prep_in(i_ + 1)
            prep_cv_out(i_)

        _chk("prep")
        wsi = [0]

        def wload(src3, a, b, slot=None):
            if slot is None:
                i = wsi[0] % 4
                wsi[0] += 1
            else:
                i = slot
            v = wsl[i][:, 0:a * b].rearrange("p (a b) -> p a b", a=a, b=b)
            dma(v, src3, ["scr"], [("w", i)])
            return v, ("w", i)

        bk_rr = {"fm": 0, "rot": 0, "g": 0, "dn": 0}
        ctr = {"xn": 0, "pb": 0, "t": 0, "R": 0, "PT": 0, "onat": 0, "ob": 0}

        def rr(name, n):
            i = ctr[name] % n
            ctr[name] += 1
            return i

        def rmsnorm_T(gi, tl, dstT3, dstkey):
            ssq, ssqk = smcol()
            xi = 0
            S.add("act", act(xn[xi][:], xg3[:, tl, :], AF.Square, accum_out=ssq),
                  [("xg", tl)], [("xn", xi), ssqk])
            _chk("rms_a")
            r, rk = rsqrt_mean(ssq, ssqk, D, RMS_EPS)
            _chk("rms_b")
            S.add("dve", ts(xn[xi][:], xg3[:, tl, :], r, None, ALU.mult), [("xg", tl), rk], [("xn", xi)])
            _chk("rms_c")
            S.add("pe", [tr(ptb3[:, c, :], xn[xi][:, c * 128:(c + 1) * 128]) for c in range(8)],
                  [("xn", xi), "ident"], [PTK])
            _chk("rms_d")
            S.add("act", act(dstT3[:, :, tl * 128:(tl + 1) * 128], ptb3, AF.Copy), [PTK], [(dstkey, tl), "hT_all"])

        for gi in range(NG):
            g0 = gi * 512
            dma(xg3, x_d[g0:g0 + 512, :].rearrange("(t p) d -> p t d", p=128),
                [], [("xg", t_) for t_ in range(4)] + ["xg_all"])
            dma(cfg[:], cf_d[:, g0:g0 + 512], [], ["cfg"])
            dma(sfg[:], sf_d[:, g0:g0 + 512], [], ["sfg"])
            dma(ctT[:], ct_d[:, gi * 128:(gi + 1) * 128], [], ["ctT"])
            dma(stT[:], st_d[:, gi * 128:(gi + 1) * 128], [], ["stT"])
            if gi > 0:
                S.add("pool", cp(bkT[:, 0:128], bkT[:, 512:640]), ["bkT"], ["bkT"])
                S.add("pool", cp(bv[:, 0:130], bv[:, 4 * 130:5 * 130]), ["bv"], ["bv"])
            for tl in range(4):
                rmsnorm_T(gi, tl, hT3, "hT")
            hkeys = [("hT", t_) for t_ in range(4)]
            _chk("rms")

            fm_banks = [0, 1, 4, 5]
            rot_banks = [2, 6]
            fm_w = {}

            def fm_chunk(c0, nk, kind, k, slot):
                if (kind, "w") not in fm_w:
                    fm_w[(kind, "w")] = wload(s_in[:, :, c0:c0 + nk * 128], 8, nk * 128, slot)
                wv, wk = fm_w[(kind, "w")]
                bi = fm_banks[bk_rr["fm"] % 4]
                bk_rr["fm"] += 1
                bo = banks[bi]
                S.add("pe", [mm(bo[:], wv[:, c, k * 128:(k + 1) * 128], hT3[:, c, :], c == 0, c == 7)
                             for c in range(8)], [wk] + hkeys, [B(bi)])
                pi = rr("pb", 2)
                S.add("act", act(pbb[pi][:], bo[:], AF.Copy), [B(bi)], [("pb", pi)])
                ri = rot_banks[bk_rr["rot"] % 2]
                bk_rr["rot"] += 1
                S.add("pe", mm(banks[ri][:], rotT[:], pbb[pi][:], True, True), [("pb", pi), "rotT"], [B(ri)])
                ti = rr("t", 2)
                S.add("dve", tt(T1[ti][:], bo[:], cfg[:], ALU.mult), [B(bi), "cfg"], [("t1", ti)])
                S.add("dve", tt(T2[ti][:], banks[ri][:], sfg[:], ALU.mult), [B(ri), "sfg"], [("t2", ti)])
                addeng = ["pool", "dve", "dve"][bk_rr["fm"] % 3]
                if kind in ("aq", "iq", "bq"):
                    base = {"aq": 0, "iq": 8, "bq": 16}[kind]
                    S.add(addeng, [tt(q3[0:64, base + k, :], T1[ti][0:64, :], T2[ti][0:64, :], ALU.add),
                                   tt(q3[64:128, base + 4 + k, :], T1[ti][64:128, :], T2[ti][64:128, :], ALU.add)],
                          [("t1", ti), ("t2", ti)], [("q", base + k), ("q", base + 4 + k)])
                else:
                    if k == 0:
                        dst, dk = akT[:, g0:g0 + 512], "akT"
                    else:
                        dst, dk = bkT[:, 128:640], "bkT"
                    S.add(addeng, tt(dst, T1[ti][:], T2[ti][:], ALU.add), [("t1", ti), ("t2", ti)], [dk])

            def tm_tile(tl, wv, wk):
                gt = gi * 4 + tl
                bo = banks[3]
                S.add("pe", [mm(bo[:, 0:N_TM], hT3[:, c, tl * 128:(tl + 1) * 128], wv[:, c, :], c == 0, c == 7)
                             for c in range(8)], [wk, ("hT", tl)], [B(3)])
                S.add("act", act(av4[:, gt, :, 0:64], bo[:, 0:128].rearrange("p (g e) -> p g e", g=2), AF.Copy),
                      [B(3)], ["av"])
                S.add("act", act(bv4[:, 1 + tl, :, 0:64], bo[:, 128:256].rearrange("p (g e) -> p g e", g=2), AF.Copy),
                      [B(3)], ["bv"])
                S.add("dve", ts(iw3[:, tl, :], bo[:, 320:328], W_SCALE, None, ALU.mult), [B(3)], [("iw", tl)])
                s1, s1k = smcol()
                S.add("dve", ts(ikx[:], bo[:, 256:320], 1.0, 0.0, ALU.mult, ALU.add, accum_out=s1),
                      [B(3)], ["ikx", s1k])
                nm, nmk = smcol()
                S.add("dve", ts(nm, s1, -1.0 / 64, None, ALU.mult), [s1k], [nmk])
                S.add("dve", ts(ikc[:], ikx[:], nm, None, ALU.add), ["ikx", nmk], ["ikc"])
                s2, s2k = smcol()
                S.add("act", act(ikx[:], ikc[:], AF.Square, accum_out=s2), ["ikc", "ikx"], ["ikx", s2k])
                r, rk = rsqrt_mean(s2, s2k, 64, LN_EPS)
                S.add("dve", stt(ikc[:], ikc[:], r, iknw[:], ALU.mult, ALU.mult), ["ikc", rk, "iknw"], ["ikc"])
                S.add("dve", tt(ikc[:], ikc[:], iknb[:], ALU.add), ["ikc", "iknb"], ["ikc"])
                c_ = ct3[:, tl, :]
                s_ = st3[:, tl, :]
                S.add("dve", [tt(ikx[:, 0:32], ikc[:, 0:32], c_, ALU.mult),
                              tt(ikx[:, 32:64], ikc[:, 32:64], c_, ALU.mult)], ["ikc", "ctT", "ikx"], ["ikx"])
                S.add("dve", [tt(dtmp[:, 0:32], ikc[:, 32:64], s_, ALU.mult),
                              tt(dtmp[:, 32:64], ikc[:, 0:32], s_, ALU.mult)], ["ikc", "stT"], ["dtmp"])
                S.add("dve", [tt(ikr[:, 0:32], ikx[:, 0:32], dtmp[:, 0:32], ALU.subtract),
                              tt(ikr[:, 32:64], ikx[:, 32:64], dtmp[:, 32:64], ALU.add)],
                      ["ikx", "dtmp"], ["ikr"])
                S.add("dve", cp(ikr[:, 64:128], ikr[:, 0:64]), ["ikr"], ["ikr"])
                S.add("pe", tr(ptb[:, 0:128], ikr[:]), ["ikr", "ident"], [PTK])
                S.add("act", act(ikT[:, gt * 128:(gt + 1) * 128], ptb[:, 0:128], AF.Copy), [PTK], ["ikT"])

            tmw = wload(s_in[:, :, C_TM:C_TM + N_TM], 8, N_TM, 3)
            chunks = ([(C_IQ, 4, "iq", k, 1) for k in range(4)] + [(C_AK, 2, "kk", k, 0) for k in range(2)]
                      + [(C_AQ, 4, "aq", k, 2) for k in range(4)] + [(C_BQ, 4, "bq", k, 1) for k in range(4)])
            per = [4, 4, 3, 3]
            ci = 0
            for tl in range(4):
                tm_tile(tl, *tmw)
                for _ in range(per[tl]):
                    fm_chunk(*chunks[ci])
                    ci += 1

            if gi == 0:
                dump("akT", akT[:, 0:512], [128, 512], BF16, "akT")
                dump("ikT", ikT[:, 0:512], [128, 512], BF16, "ikT")
                dump("av", av[:, 0:520], [128, 520], BF16, "av")
                dump("iw", iw[:], [128, 32], F32, [("iw", t_) for t_ in range(4)])

            _chk("stage1")
            def idx_phase(tl):
                gt = gi * 4 + tl
                nk = (gt + 1) * 128
                diag3 = diag3s[tl % 2]
                dgk = ("diag", tl % 2)
                sc_ = scs[tl % 2]
                sck = ("scores", tl % 2)
                S.add("pool", [ts(diag3[:, h, :], ident[:], iw3[:, tl, h:h + 1], None, ALU.mult) for h in range(8)],
                      [("iw", tl), "ident"], [dgk])
                nchunk = (nk + 511) // 512
                for c in range(nchunk):
                    c0 = c * 512
                    ncol = min(512, nk - c0)
                    last = (c == nchunk - 1)
                    pend = []

                    def dmm(hh, rj):
                        fns = [mm(banks[2][:, 0:ncol], diag3[:, hh, :], Rb[rj][:, 0:ncol], hh == 0, (hh == 7) and not last)]
                        rd_ = [("R", rj), dgk]
                        if hh == 7 and last:
                            fns.append(mm(banks[2][:, ncol - 128:ncol], ident[:], cmask[:], False, True))
                            rd_ += ["ident", "cmask"]
                        S.add("pe", fns, rd_, [B(2)])

                    for h in range(8):
                        half, kq = h % 2, h // 2
                        sbk = h % 2
                        S.add("pe", mm(banks[sbk][:, 0:ncol],
                                       q3[:, 8 + half * 4 + kq, tl * 128:(tl + 1) * 128],
                                       ikT[:, c0:c0 + ncol], True, True),
                              [("q", 8 + half * 4 + kq), "ikT"], [B(sbk)])
                        ri = rr("R", 3)
                        S.add("act", act(Rb[ri][:, 0:ncol], banks[sbk][:, 0:ncol], AF.Relu), [B(sbk)], [("R", ri)])
                        pend.append((h, ri))
                        if len(pend) == 2:
                            dmm(*pend.pop(0))
                    while pend:
                        dmm(*pend.pop(0))
                    S.add("act", act(sc_[:, c0:c0 + ncol], banks[2][:, 0:ncol], AF.Copy), [B(2)], [sck])
                return nk

            def thr_phase(tl):
                gt = gi * 4 + tl
                nk = (gt + 1) * 128
                thr, thrk = smcol()
                sc_ = scs[tl % 2]
                sck = ("scores", tl % 2)
                if gt < 2:
                    S.add("dve", lambda e: e.memset(thr, -1.0e29), [], [thrk])
                else:
                    nd = nk - 128
                    aa, aak = smcol()
                    dmx, dmxk = smcol()
                    hi, hik = smcol()
                    S.add("dve", lambda e: e.tensor_reduce(aa, sc_[:, 0:nd], AX.X, ALU.max, apply_absolute_value=True),
                          [sck], [aak])
                    S.add("dve", lambda e: e.tensor_reduce(dmx, sc_[:, nd:nk], AX.X, ALU.max), [sck], [dmxk])
                    S.add("dve", tt(hi, aa, dmx, ALU.max), [aak, dmxk], [hik])
                    w0, w0k = smcol()
                    S.add("dve", stt(w0, hi, 1.0, aa, ALU.add, ALU.add), [hik, aak], [w0k])
                    S.add("dve", ts(hwk[:], pow2[:], w0, None, ALU.mult), ["pow2", w0k], ["hwk"])
                    mid, midk = smcol()
                    S.add("dve", tt(mid, hwk[:, 0:1], aa, ALU.subtract), [aak, "hwk"], [midk])
                    cnt, cntk = smcol()
                    stp, stpk = smcol()
                    for it in range(NIT):
                        S.add("dve", ts(junk[:, 0:nk], sc_[:, 0:nk], mid, 0.0, ALU.is_ge, ALU.add, accum_out=cnt),
                              [sck, midk], [JKK, cntk])
                        S.add("dve", ts(stp, cnt, float(TOPK), hwk[:, it:it + 1], ALU.is_ge, ALU.mult),
                              [cntk, "hwk"], [stpk])
                        S.add("dve", stt(mid, stp, hwk[:, it + 1:it + 2], mid, ALU.subtract, ALU.add),
                              [stpk, "hwk", midk], [midk])
                    S.add("dve", tt(thr, mid, hwk[:, NIT:NIT + 1], ALU.subtract), [midk, "hwk"], [thrk])
                S.add("dve", ts(mbs[tl % 2][:, 0:nk], sc_[:, 0:nk], thr, NEG, ALU.is_lt, ALU.mult),
                      [sck, thrk], [("mb", tl % 2)])

            def attn_core(tl, blocks, qbase, kfun, vfun, biasfun, sink, ochunk0):
                first = [True, True]
                pend = []

                def pv(j, g, pi):
                    vap, vk = vfun(j, g)
                    fns = []
                    for hh in range(4):
                        fns.append(mm(banks[5 + g][:, hh * 65:(hh + 1) * 65], PTb[pi][:, hh * 128:(hh + 1) * 128],
                                      vap, first[g] and hh == 0, False))
                    first[g] = False
                    S.add("pe", fns, [("PT", pi), vk], [B(5 + g)])

                stb = [3, 4]
                n = 0
                for j in blocks:
                    for g in range(2):
                        kap, kk = kfun(j, g)
                        bl, br, bkeys = biasfun(j)
                        sbi = stb[n % 2]
                        n += 1
                        S.add("pe", [mm(banks[sbi][:], kap, q3[:, qbase + g * 4:qbase + g * 4 + 4,
                                                                tl * 128:(tl + 1) * 128], True, False),
                                     mm(banks[sbi][:], bl, br, False, True)],
                              [kk] + [("q", qbase + g * 4 + k) for k in range(4)] + bkeys, [B(sbi)])
                        pi = rr("PT", 3)
                        S.add("act", act(PTb[pi][:], banks[sbi][:], AF.Exp, scale=0.125), [B(sbi)], [("PT", pi)])
                        pend.append((j, g, pi))
                        if len(pend) == 2:
                            pv(*pend.pop(0))
                while pend:
                    pv(*pend.pop(0))
                oi = rr("onat", 3)
                for g in range(2):
                    o3 = banks[5 + g][:, 0:260].rearrange("p (h e) -> p h e", e=65)
                    if sink:
                        S.add("dve", tt(rd[:, g * 4:(g + 1) * 4], o3[:, :, 64], esink[:, g * 4:(g + 1) * 4], ALU.add),
                              [B(5 + g), "esink"], [("rd", g)])
                        S.add("dve", lambda e, g=g: e.reciprocal(rd[:, g * 4:(g + 1) * 4], rd[:, g * 4:(g + 1) * 4]),
                              [("rd", g)], [("rd", g)])
                    else:
                        S.add("dve", lambda e, g=g, o3=o3: e.reciprocal(rd[:, g * 4:(g + 1) * 4], o3[:, :, 64]),
                              [B(5 + g)], [("rd", g)])
                    S.add("dve", [ts(onat[oi][:, (g * 4 + hh) * 64:(g * 4 + hh + 1) * 64], o3[:, hh, 0:64],
                                     rd[:, g * 4 + hh:g * 4 + hh + 1], None, ALU.mult) for hh in range(4)],
                          [B(5 + g), ("rd", g)], [("onat", oi)])
                def finish():
                    S.add("pe", [tr(ptb3[:, c, :], onat[oi][:, c * 128:(c + 1) * 128]) for c in range(4)],
                          [("onat", oi), "ident"], [PTK])
                    S.add("act", act(oT3[:, ochunk0:ochunk0 + 4, tl * 128:(tl + 1) * 128], ptb3[:, 0:4, :], AF.Copy),
                          [PTK], [("oT", tl), "oT_all"])
                return finish

            def swa_phase(tl):
                gt = gi * 4 + tl
                blocks = ([0] if gt > 0 else []) + [1]

                def kfun(j, g):
                    c0 = (tl + j) * 128
                    return bkT[:, c0:c0 + 128], "bkT"

                def vfun(j, g):
                    return bv4[:, tl + j, g, :], "bv"

                def biasfun(j):
                    return ident[:], (swap[:] if j == 0 else swac[:]), ["ident", "swap", "swac"]

                return attn_core(tl, blocks, 16, kfun, vfun, biasfun, True, 4)

            def dsa_phase(tl):
                gt = gi * 4 + tl

                def kfun(j, g):
                    return akT[:, j * 128:(j + 1) * 128], "akT"

                def vfun(j, g):
                    return av4[:, j, g, :], "av"

                def biasfun(j):
                    return mbs[tl % 2][:, j * 128:(j + 1) * 128], ident4[:], [("mb", tl % 2), "ident4"]

                return attn_core(tl, list(range(gt + 1)), 0, kfun, vfun, biasfun, False, 0)

            idx_phase(0)
            _chk("idx0")
            thr_phase(0)
            _chk("thr0")
            pending_fin = None
            for tl in range(4):
                if tl < 3:
                    idx_phase(tl + 1)
                fin_swa = swa_phase(tl)
                fin_swa()
                if pending_fin is not None:
                    pending_fin()
                    pending_fin = None
                if tl < 3:
                    thr_phase(tl + 1)
                if gi == 0 and tl == 2:
                    dump("scores", scs[1][:, 0:512], [128, 512], F32, ("scores", 1))
                    dump("mb", mbs[1][:, 0:512], [128, 512], BF16, ("mb", 1))
                pending_fin = dsa_phase(tl)
            pending_fin()
            if gi == 0:
                dump("oT", oT[:], [128, 4096], BF16, [("oT", t_) for t_ in range(4)])

            _chk("stage2")
            okeys = [("oT", t_) for t_ in range(4)]
            g_banks = [0, 1, 2, 3, 4, 5, 6]
            for half in range(2):
                if half == 0:
                    wba_v, wba_k = wload(s_ba, 4, D, 1)
                    wbb_v, wbb_k = wload(s_bb, 4, D, 2)
                wga_v, wga_k = wload(s_in[:, :, C_GA + half * 512:C_GA + (half + 1) * 512], 8, 512, 3)
                wgb_v, wgb_k = wload(s_in[:, :, C_GB + half * 512:C_GB + (half + 1) * 512], 8, 512, 0)
                for cc in range(4):
                    c = half * 4 + cc
                    bs = []
                    for _ in range(4):
                        bs.append(g_banks[bk_rr["g"] % 7])
                        bk_rr["g"] += 1
                    bya, byb, bga, bgb = bs
                    S.add("pe", [mm(banks[bya][:], wba_v[:, e_, c * 128:(c + 1) * 128], oT3[:, e_, :], e_ == 0, e_ == 3)
                                 for e_ in range(4)], [wba_k] + okeys, [B(bya)])
                    S.add("pe", [mm(banks[byb][:], wbb_v[:, e_, c * 128:(c + 1) * 128], oT3[:, 4 + e_, :], e_ == 0, e_ == 3)
                                 for e_ in range(4)], [wbb_k] + okeys, [B(byb)])
                    S.add("pe", [mm(banks[bga][:], wga_v[:, d_, cc * 128:(cc + 1) * 128], hT3[:, d_, :], d_ == 0, d_ == 7)
                                 for d_ in range(8)], [wga_k] + hkeys, [B(bga)])
                    S.add("pe", [mm(banks[bgb][:], wgb_v[:, d_, cc * 128:(cc + 1) * 128], hT3[:, d_, :], d_ == 0, d_ == 7)
                                 for d_ in range(8)], [wgb_k] + hkeys, [B(bgb)])
                    ti = rr("t", 2)
                    S.add("act", act(T1[ti][:], banks[bga][:], AF.Tanh, scale=0.5), [B(bga)], [("t1", ti)])
                    S.add("act", act(T2[ti][:], banks[bgb][:], AF.Tanh, scale=0.5), [B(bgb)], [("t2", ti)])
                    S.add("dve", stt(T1[ti][:], T1[ti][:], 1.0, banks[bya][:], ALU.add, ALU.mult),
                          [("t1", ti), B(bya)], [("t1", ti)])
                    S.add("dve", stt(T2[ti][:], T2[ti][:], 1.0, banks[byb][:], ALU.add, ALU.mult),
                          [("t2", ti), B(byb)], [("t2", ti)])
                    S.add("pool", tt(mixT3[:, c, :], T1[ti][:], T2[ti][:], ALU.add),
                          [("t1", ti), ("t2", ti)], [("mix", c), ("scores", 1)])
            mkeys = [("mix", c) for c in range(8)] + [("scores", 1)]
            for half in range(2):
                wo_v, wo_k = wload(s_out[:, :, half * 512:(half + 1) * 512], 8, 512, 1 + half)
                for tl in range(4):
                    bi = g_banks[bk_rr["g"] % 7]
                    bk_rr["g"] += 1
                    S.add("pe", [mm(banks[bi][:], mixT3[:, d_, tl * 128:(tl + 1) * 128], wo_v[:, d_, :], d_ == 0, d_ == 7)
                                 for d_ in range(8)], [wo_k] + mkeys, [B(bi)])
                    S.add("dve", tt(xg3[:, tl, half * 512:(half + 1) * 512], xg3[:, tl, half * 512:(half + 1) * 512],
                                    banks[bi][:], ALU.add), [B(bi), ("xg", tl)], [("xg", tl)])
            if gi == 0:
                dump("x1", xg[:], [128, 4096], F32, [("xg", t_) for t_ in range(4)])

            _chk("stage3")
            for tl in range(4):
                rmsnorm_T(gi, tl, hT3, "hT")
            def mlp_up(hc):
                wu_v, wu_k = wload(s_up[:, :, hc * 512:(hc + 1) * 512], 8, 512, [3, 1][hc % 2])
                wd = wload(s_dn[:, hc * 4:(hc + 1) * 4, :], 4, D, [0, 2][hc % 2])
                u3 = u2Ts[hc % 2]
                for fc in range(4):
                    bi = [0, 1][fc % 2]
                    S.add("pe", [mm(banks[bi][:], wu_v[:, d_, fc * 128:(fc + 1) * 128], hT3[:, d_, :], d_ == 0, d_ == 7)
                                 for d_ in range(8)], [wu_k] + hkeys, [B(bi)])
                    ti = rr("t", 2)
                    S.add("act", act(T1[ti][:], banks[bi][:], AF.Relu), [B(bi)], [("t1", ti)])
                    S.add("pool", tt(u3[:, fc, :], T1[ti][:], T1[ti][:], ALU.mult), [("t1", ti)], [("u2", 0, fc)])
                return wd

            def mlp_down(hc, wd):
                wd_v, wd_k = wd
                u3 = u2Ts[hc % 2]
                ukeys = [("u2", 0, fc) for fc in range(4)]
                for tl in range(4):
                    for half in range(2):
                        bi = [2, 3, 4, 5, 6][bk_rr["dn"] % 5]
                        bk_rr["dn"] += 1
                        S.add("pe", [mm(banks[bi][:], u3[:, fc, tl * 128:(tl + 1) * 128],
                                        wd_v[:, fc, half * 512:(half + 1) * 512], fc == 0, fc == 3) for fc in range(4)],
                              [wd_k] + ukeys, [B(bi)])
                        S.add("dve", tt(xg3[:, tl, half * 512:(half + 1) * 512], xg3[:, tl, half * 512:(half + 1) * 512],
                                        banks[bi][:], ALU.add), [B(bi), ("xg", tl)], [("xg", tl)])

            for hc in range(8):
                mlp_down(hc, mlp_up(hc))
            for tl in range(4):
                ssq, ssqk = smcol()
                oi = 0
                S.add("act", act(obuf[oi], xg3[:, tl, :], AF.Square, accum_out=ssq), [("xg", tl)], [OBK, ssqk])
                r, rk = rsqrt_mean(ssq, ssqk, D, RMS_EPS)
                S.add("dve", stt(obuf[oi], xg3[:, tl, :], r, fnw[:], ALU.mult, ALU.mult),
                      [("xg", tl), rk, "fnw"], [OBK])
                r0 = g0 + tl * 128
                dma(out_d[r0:r0 + 128, :], obuf[oi], [OBK], [])

    except _Stop:
        pass

    def emit(name, eng):
        for fns, waits, me, is_dma in S.ops[name]:
            for k, v in waits:
                eng.wait_ge(sems[k], v)
            ins = None
            for f in fns:
                ins = f(eng)
            ins.then_inc(sems[me[0]], 16 if is_dma else 1)
        if name == "sp":
            for i in range(NDMA):
                if S.dma_val[i] > 0:
                    eng.wait_ge(sems[("dma", i)], S.dma_val[i])

    with nc.Block() as block:
        @block.sync
        def _(e):
            emit("sp", e)

        @block.tensor
        def _(e):
            emit("pe", e)

        @block.scalar
        def _(e):
            emit("act", e)

        @block.vector
        def _(e):
            emit("dve", e)

        @block.gpsimd
        def _(e):
            emit("pool", e)
    print("SBUF bytes remaining:", nc.sbuf_bytes_remaining, "ops:", {k: len(v) for k, v in S.ops.items()})
    es.close()
    return nc, dbg_d


def _bf(a):
    return np.asarray(a, dtype=np.float32).astype(ml_dtypes.bfloat16)


def host_consts(L):
    NT = L // 128
    inv = (1.0 / (np.float32(10000.0) ** (np.arange(0, 64, 2, dtype=np.float32) / np.float32(64)))).astype(np.float32)
    ang = (np.arange(L, dtype=np.float32)[:, None] * inv[None, :]).astype(np.float32)
    cos, sin = np.cos(ang).astype(np.float32), np.sin(ang).astype(np.float32)
    p = np.arange(128)
    cf = np.ascontiguousarray(cos[:, p % 32].T)
    sf = np.ascontiguousarray(sin[:, p % 32].T)
    ct = np.ascontiguousarray(cos.reshape(NT, 128, 32).transpose(1, 0, 2).reshape(128, NT * 32))
    st = np.ascontiguousarray(sin.reshape(NT, 128, 32).transpose(1, 0, 2).reshape(128, NT * 32))
    ident = np.eye(128, dtype=np.float32)
    rot = np.zeros((128, 128), np.float32)
    for m in range(128):
        if (m % 64) < 32:
            rot[m + 32, m] = -1.0
        else:
            rot[m - 32, m] = 1.0
    sp = np.arange(128)[:, None]
    tp = np.arange(128)[None, :]
    cur = np.where(sp <= tp, 0.0, NEG).astype(np.float32)
    prev = np.where(sp > tp, 0.0, NEG).astype(np.float32)
    cmask = np.where(np.arange(128)[None, :] <= np.arange(128)[:, None], 0.0, SNEG).astype(np.float32)
    pow2 = np.tile((0.5 ** np.arange(1, NIT + 2, dtype=np.float64)).astype(np.float32)[None, :], (128, 1))
    return {
        "ropeCf": cf, "ropeSf": sf, "ropeCt": ct, "ropeSt": st,
        "ident": _bf(ident), "ident4": _bf(np.tile(ident, (1, 4))), "rotT": _bf(rot),
        "swa_cur": _bf(np.tile(cur, (1, 4))), "swa_prev": _bf(np.tile(prev, (1, 4))),
        "cmask": _bf(cmask), "pow2": pow2,
    }


def permute_w_in(w):
    a_q, a_k, a_v = w[:, 0:512], w[:, 512:640], w[:, 640:768]
    i_q, i_k, i_w = w[:, 768:1280], w[:, 1280:1344], w[:, 1344:1352]
    b_q, b_k, b_v = w[:, 1352:1864], w[:, 1864:1992], w[:, 1992:2120]
    g_a, g_b = w[:, 2120:3144], w[:, 3144:4168]

    def grp(q):
        cols = []
        for k in range(4):
            cols.append(q[:, k * 64:(k + 1) * 64])
            cols.append(q[:, (4 + k) * 64:(5 + k) * 64])
        return np.concatenate(cols, axis=1)

    return np.ascontiguousarray(np.concatenate(
        [grp(a_q), i_q, grp(b_q), a_k, b_k, g_a, g_b, a_v, b_v, i_k, i_w], axis=1))


_CACHE = {}


def make_in_maps(L, x, attn_norm_w, w_in, idx_k_norm_w, idx_k_norm_b, sinks, w_branch_a, w_branch_b,
                 w_out, mlp_norm_w, w_up, w_down, final_norm_w):
    f = lambda a: np.ascontiguousarray(np.asarray(a, dtype=np.float32))
    shared = dict(host_consts(L))
    shared.update({
        "w_in": permute_w_in(f(w_in[0])),
        "w_ba": f(w_branch_a[0]), "w_bb": f(w_branch_b[0]), "w_out": f(w_out[0]),
        "w_up": f(w_up[0]), "w_dn": f(w_down[0]),
        "anw": f(np.asarray(attn_norm_w[0]).reshape(8, 128).T),
        "mnw": f(np.asarray(mlp_norm_w[0]).reshape(8, 128).T),
        "fnw": f(np.tile(np.asarray(final_norm_w).reshape(1, D), (128, 1))),
        "iknw": f(np.tile(np.asarray(idx_k_norm_w[0]).reshape(1, 64), (128, 1))),
        "iknb": f(np.tile(np.asarray(idx_k_norm_b[0]).reshape(1, 64), (128, 1))),
        "sinks": f(np.tile(np.asarray(sinks[0]).reshape(1, 8), (128, 1))),
    })
    x = np.asarray(x, dtype=np.float32)
    maps = []
    for b in range(x.shape[0]):
        m = dict(shared)
        m["x"] = np.ascontiguousarray(x[b, :L])
        maps.append(m)
    return maps


def run(L, inputs, dbg=(), n_cores=8, trace=False):
    key = (L, tuple(dbg))
    nc, dbg_d = build_program(L, dbg)
    maps = make_in_maps(L, **inputs)[:n_cores]
    res = run_bass_kernel_spmd(nc, maps, core_ids=list(range(len(maps))), trace=trace)
    return res


def kernel(**inputs):
    L = 4096
    res = run(L, inputs)
    out = np.stack([np.asarray(r["out"], dtype=np.float32) for r in res.results], axis=0)
    return out
```

```python
import numpy as np
import ml_dtypes
from contextlib import ExitStack
import concourse.bass as bass
import concourse.mybir as mybir
from concourse.bass_utils import run_bass_kernel_spmd

F32 = mybir.dt.float32
BF16 = mybir.dt.bfloat16
ALU = mybir.AluOpType
AF = mybir.ActivationFunctionType
AX = mybir.AxisListType

D = 1024
DIN = 4168
DFF = 4096
TOPK = 256
NEG = -30000.0
SNEG = -1.0e30
W_SCALE = (8 ** -0.5) * (64 ** -0.5)
NIT = 18
NDMA = 12
RMS_EPS = 1e-6
LN_EPS = 1e-6

C_AQ, C_IQ, C_BQ, C_AK, C_BK = 0, 512, 1024, 1536, 1664
C_GA, C_GB = 1792, 2816
C_TM = 3840
N_TM = 328


class _Stop(Exception):
    pass


STOP_AFTER = None


def _chk(stage):
    if STOP_AFTER == stage:
        raise _Stop()


class Sched:
    ENG = ("sp", "pe", "act", "dve", "pool")

    def __init__(self):
        self.ops = {e: [] for e in self.ENG}
        self.count = {e: 0 for e in self.ENG}
        self.waited = {e: {} for e in self.ENG}
        self.buf_w = {}
        self.buf_r = {}
        self.dma_val = [0] * NDMA
        self.next_dma = 0

    def add(self, eng, fns, reads=(), writes=(), dma=False):
        if not isinstance(fns, (list, tuple)):
            fns = [fns]
        writes = list(writes) + [b for b in reads if isinstance(b, tuple) and b[0] == "ps" and b not in writes]
        deps = {}

        def need(k, v, kind):
            if k == eng and eng == "pe":
                return
            if deps.get(k, 0) < v:
                deps[k] = v

        for b in reads:
            w = self.buf_w.get(b)
            if w:
                need(w[0], w[1], "raw")
        for b in writes:
            w = self.buf_w.get(b)
            if w:
                need(w[0], w[1], "waw")
            for k, v in self.buf_r.get(b, {}).items():
                need(k, v, "war")
        if dma:
            s = self.next_dma
            self.next_dma = (s + 1) % NDMA
            if self.dma_val[s] > 0:
                need(("dma", s), self.dma_val[s], "raw")
            self.dma_val[s] += 16
            me = (("dma", s), self.dma_val[s])
        else:
            self.count[eng] += 1
            me = (eng, self.count[eng])
        waits = []
        for k, v in deps.items():
            if self.waited[eng].get(k, 0) >= v:
                continue
            self.waited[eng][k] = v
            waits.append((k, v))
        self.ops[eng].append((list(fns), waits, me, dma))
        for b in reads:
            d = self.buf_r.setdefault(b, {})
            if d.get(me[0], 0) < me[1]:
                d[me[0]] = me[1]
        for b in writes:
            self.buf_w[b] = me
            self.buf_r[b] = {}
        return me


def build_program(L, dbg=()):
    NT, NG = L // 128, L // 512
    nc = bass.Bass("TRN2", target_bir_lowering=False)

    def din(name, shape, dt=F32):
        return nc.dram_tensor(name, list(shape), dt, kind="ExternalInput").ap()

    x_d = din("x", [L, D])
    win_d = din("w_in", [D, DIN])
    wba_d = din("w_ba", [512, D])
    wbb_d = din("w_bb", [512, D])
    wout_d = din("w_out", [D, D])
    wup_d = din("w_up", [D, DFF])
    wdn_d = din("w_dn", [DFF, D])
    anw_d = din("anw", [128, 8])
    mnw_d = din("mnw", [128, 8])
    fnw_d = din("fnw", [128, D])
    iknw_d = din("iknw", [128, 64])
    iknb_d = din("iknb", [128, 64])
    sinks_d = din("sinks", [128, 8])
    cf_d = din("ropeCf", [128, L])
    sf_d = din("ropeSf", [128, L])
    ct_d = din("ropeCt", [128, NT * 32])
    st_d = din("ropeSt", [128, NT * 32])
    ident_d = din("ident", [128, 128], BF16)
    ident4_d = din("ident4", [128, 512], BF16)
    rotT_d = din("rotT", [128, 128], BF16)
    swac_d = din("swa_cur", [128, 512], BF16)
    swap_d = din("swa_prev", [128, 512], BF16)
    cmask_d = din("cmask", [128, 128], BF16)
    pow2_d = din("pow2", [128, NIT + 1])
    out_d = nc.dram_tensor("out", [L, D], F32, kind="ExternalOutput").ap()
    dbg_d = {}

    def dscr(name, shape):
        return nc.dram_tensor(name, list(shape), BF16, kind="Internal").ap()

    s_in = dscr("s_in", [128, 8, DIN])
    s_ba = dscr("s_ba", [128, 4, D])
    s_bb = dscr("s_bb", [128, 4, D])
    s_out = dscr("s_out", [128, 8, D])
    s_up = dscr("s_up", [128, 8, DFF])
    s_dn = dscr("s_dn", [128, 32, D])

    S = Sched()
    es = ExitStack()

    def sb(name, shape, dt):
        return es.enter_context(nc.sbuf_tensor("sb_" + name, list(shape), dt))

    LS = max(L, 4096)
    akT = sb("akT", [128, L], BF16)
    ikT = sb("ikT", [128, L], BF16)
    av = sb("av", [128, NT * 130], BF16)
    bkT = sb("bkT", [128, 640], BF16)
    bv = sb("bv", [128, 5 * 130], BF16)
    xg = sb("xg", [128, 4096], F32)
    hT = sb("hT", [128, 4096], BF16)
    oT = sb("oT", [128, 4096], BF16)
    qall = sb("qall", [128, 24 * 512], BF16)
    iw = sb("iw", [128, 32], F32)
    scs = [sb(f"scores{i}", [128, LS], F32) for i in range(2)]
    scores = scs[0]
    mbs = [sb(f"mb{i}", [128, L], BF16) for i in range(2)]
    Rb = [sb(f"R{i}", [128, 512], BF16) for i in range(3)]
    PTb = [sb(f"PT{i}", [128, 512], BF16) for i in range(3)]
    diags = [sb(f"diag{i}", [128, 1024], BF16) for i in range(2)]
    wsl = [sb(f"wsl{i}", [128, 4096], BF16) for i in range(4)]
    cfg = sb("cfg", [128, 512], F32)
    sfg = sb("sfg", [128, 512], F32)
    ctT = sb("ctT", [128, 4 * 32], F32)
    stT = sb("stT", [128, 4 * 32], F32)
    fnw = sb("fnw", [128, D], F32)
    iknw = sb("iknw", [128, 64], F32)
    iknb = sb("iknb", [128, 64], F32)
    esink = sb("esink", [128, 8], F32)
    anw = sb("anw", [128, 8], F32)
    mnw = sb("mnw", [128, 8], F32)
    ident = sb("ident", [128, 128], BF16)
    ident4 = sb("ident4", [128, 512], BF16)
    rotT = sb("rotT", [128, 128], BF16)
    swac = sb("swac", [128, 512], BF16)
    swap = sb("swap", [128, 512], BF16)
    cmask = sb("cmask", [128, 128], BF16)
    pow2 = sb("pow2", [128, NIT + 1], F32)
    neghalf = sb("neghalf", [128, 1], F32)
    T1 = [sb(f"t1_{i}", [128, 512], F32) for i in range(2)]
    T2 = [sb(f"t2_{i}", [128, 512], F32) for i in range(2)]
    pbb = [sb(f"pb{i}", [128, 512], BF16) for i in range(2)]
    xn = [sb(f"xn{i}", [128, D], BF16) for i in range(1)]
    onat = [sb(f"onat{i}", [128, 512], BF16) for i in range(3)]
    obuf = [scs[0][:, 0:D]]
    OBK = ("scores", 0)
    junk = wsl[0][:].bitcast(mybir.dt.uint8)
    JKK = ("w", 0)
    u2Tt = [sb(f"u2T{i}", [128, 2048], BF16) for i in range(1)] * 2
    sm = sb("sm", [128, 64], F32)
    hwk = sb("hwk", [128, NIT + 1], F32)
    ikx = sb("ikx", [128, 64], F32)
    ikc = sb("ikc", [128, 64], F32)
    ikr = sb("ikr", [128, 128], BF16)
    dtmp = sb("dtmp", [128, 128], F32)
    rd = sb("rd", [128, 8], F32)

    banks = [es.enter_context(nc.psum_tensor(f"ps{i}", [128, 512], F32)) for i in range(7)]
    ptb = es.enter_context(nc.psum_tensor("ptb", [128, 1024], BF16))
    PTK = ("ps", "pt")

    def B(i):
        return ("ps", i)

    sems = {}
    for e in Sched.ENG:
        sems[e] = es.enter_context(nc.semaphore(f"sem_{e}"))
    for i in range(NDMA):
        sems[("dma", i)] = es.enter_context(nc.semaphore(f"sem_dma{i}"))

    def v3(t, a, b):
        return t[:].rearrange("p (a b) -> p a b", a=a, b=b)

    xg3 = v3(xg, 4, D)
    hT3 = v3(hT, 8, 512)
    oT3 = v3(oT, 8, 512)
    q3 = v3(qall, 24, 512)
    mixT3 = scs[1][:].bitcast(BF16)[:, 0:4096].rearrange("p (a b) -> p a b", a=8, b=512)
    av4 = av[:].rearrange("p (t g e) -> p t g e", g=2, e=65)
    bv4 = bv[:].rearrange("p (t g e) -> p t g e", g=2, e=65)
    diag3s = [v3(d_, 8, 128) for d_ in diags]
    u2Ts = [v3(t_, 4, 512) for t_ in u2Tt]
    ptb3 = ptb[:].rearrange("p (a b) -> p a b", a=8, b=128)
    iw3 = v3(iw, 4, 8)
    ct3 = ctT[:].rearrange("p (t f) -> p t f", f=32)
    st3 = stT[:].rearrange("p (t f) -> p t f", f=32)

    def dma(out, in_, reads, writes):
        return S.add("sp", lambda e: e.dma_start(out=out, in_=in_), reads, writes, dma=True)

    def mm(out, lhsT, rhs, start, stop):
        return lambda e: e.matmul(out, lhsT, rhs, start=start, stop=stop, skip_group_check=True)

    def tr(out, in_):
        return lambda e: e.transpose(out, in_, ident[:])

    def act(out, in_, func, scale=1.0, accum_out=None):
        if accum_out is None:
            return lambda e: e.activation(out, in_, func, scale=scale)
        return lambda e: e.activation(out, in_, func, scale=scale, accum_out=accum_out)

    def ts(out, in0, s1, s2, op0, op1=None, accum_out=None):
        if op1 is None:
            return lambda e: e.tensor_scalar(out, in0, s1, None, op0)
        if accum_out is None:
            return lambda e: e.tensor_scalar(out, in0, s1, s2, op0, op1)
        return lambda e: e.tensor_scalar(out, in0, s1, s2, op0, op1, accum_out)

    def tt(out, in0, in1, op):
        return lambda e: e.tensor_tensor(out, in0, in1, op)

    def stt(out, in0, scalar, in1, op0, op1):
        return lambda e: e.scalar_tensor_tensor(out, in0, scalar, in1, op0, op1)

    def cp(out, in_):
        return lambda e: e.tensor_copy(out, in_)

    smi = [0]

    def smcol():
        i = smi[0] % 64
        smi[0] += 1
        return sm[:, i:i + 1], ("sm", i)

    def rsqrt_mean(ssq_ap, ssq_key, n, eps):
        a, ak = smcol()
        r, rk = smcol()
        S.add("dve", ts(a, ssq_ap, 1.0 / n, eps, ALU.mult, ALU.add), [ssq_key], [ak])
        S.add("pool", tt(r, a, neghalf[:], ALU.pow), [ak, "neghalf"], [rk])
        return r, rk

    def dump(name, ap, shape, dt, key):
        if name not in dbg:
            return
        d_ = nc.dram_tensor("dbg_" + name, list(shape), dt, kind="ExternalOutput").ap()
        dbg_d[name] = d_
        dma(d_, ap, [key] if not isinstance(key, list) else key, [])

    try:
        for t, d_, k in ((ident, ident_d, "ident"), (ident4, ident4_d, "ident4"), (rotT, rotT_d, "rotT"),
                         (swac, swac_d, "swac"), (swap, swap_d, "swap"), (cmask, cmask_d, "cmask"),
                         (pow2, pow2_d, "pow2"),
                         (fnw, fnw_d, "fnw"), (iknw, iknw_d, "iknw"), (iknb, iknb_d, "iknb"),
                         (esink, sinks_d, "esink"), (anw, anw_d, "anw"), (mnw, mnw_d, "mnw")):
            dma(t[:], d_, [], [k])
        S.add("dve", lambda e: e.memset(neghalf[:], -0.5), [], ["neghalf"])
        S.add("act", act(esink[:], esink[:], AF.Exp), ["esink"], ["esink"])
        S.add("pool", lambda e: e.memset(av[:], 1.0), [], ["av"])
        S.add("pool", lambda e: e.memset(bv[:], 1.0), [], ["bv"])
        S.add("pool", lambda e: e.memset(bkT[:], 0.0), [], ["bkT"])
        S.add("pool", lambda e: e.memset(qall[:], 0.0), [], [("q", i_) for i_ in range(24)])

        _chk("consts")
        stg = [xg, scores]
        stgk = ["xg_all", ("scores", 0)]
        cvo = [hT, oT]
        cvok = ["hT_all", "oT_all"]
        prep_i = [0]

        prep_list = []

        def prep_block(src3, dst3, nch, ncols, fold, scale=None):
            prep_list.append((src3, dst3, nch, ncols, fold, scale))

        def prep_in(i):
            src3, dst3, nch, ncols, fold, scale = prep_list[i]
            sv = stg[i % 2][:, 0:nch * ncols].rearrange("p (a b) -> p a b", a=nch, b=ncols)
            dma(sv, src3, [], [stgk[i % 2]])

        def prep_cv_out(i):
            src3, dst3, nch, ncols, fold, scale = prep_list[i]
            j = i % 2
            sv = stg[j][:, 0:nch * ncols].rearrange("p (a b) -> p a b", a=nch, b=ncols)
            ov = cvo[j][:, 0:nch * ncols].rearrange("p (a b) -> p a b", a=nch, b=ncols)
            if fold is not None:
                fns = [ts(ov[:, c, :], sv[:, c, :], fold[:, c:c + 1], None, ALU.mult) for c in range(nch)]
                S.add("dve", fns, [stgk[j], "anw", "mnw"], [cvok[j]])
            elif scale is not None:
                S.add("act", act(cvo[j][:, 0:nch * ncols], stg[j][:, 0:nch * ncols], AF.Identity, scale=scale),
                      [stgk[j]], [cvok[j]])
            else:
                S.add("act", act(cvo[j][:, 0:nch * ncols], stg[j][:, 0:nch * ncols], AF.Copy),
                      [stgk[j]], [cvok[j]])
            dma(dst3, ov, [cvok[j]], ["scr"])

        win3 = win_d.rearrange("(c p) n -> p c n", p=128)
        for c0 in range(0, DIN, 512):
            c1 = min(DIN, c0 + 512)
            prep_block(win3[:, :, c0:c1], s_in[:, :, c0:c1], 8, c1 - c0, anw)
        prep_block(wba_d.rearrange("(c p) n -> p c n", p=128), s_ba, 4, D, None)
        prep_block(wbb_d.rearrange("(c p) n -> p c n", p=128), s_bb, 4, D, None)
        wout3 = wout_d.rearrange("(c p) n -> p c n", p=128)
        for c0 in range(0, D, 512):
            prep_block(wout3[:, :, c0:c0 + 512], s_out[:, :, c0:c0 + 512], 8, 512, None, scale=0.5)
        wup3 = wup_d.rearrange("(c p) n -> p c n", p=128)
        for c0 in range(0, DFF, 512):
            prep_block(wup3[:, :, c0:c0 + 512], s_up[:, :, c0:c0 + 512], 8, 512, mnw)
        wdn3 = wdn_d.rearrange("(c p) n -> p c n", p=128)
        for c0 in range(0, 32, 4):
            prep_block(wdn3[:, c0:c0 + 4, :], s_dn[:, c0:c0 + 4, :], 4, D, None)
        prep_in(0)
        for i_ in range(len(prep_list)):
            if i_ + 1 < len(prep_list):
                prep_in(i_ + 1)
            prep_cv_out(i_)

        _chk("prep")
        wsi = [0]

        def wload(src3, a, b, slot=None):
            if slot is None:
                i = wsi[0] % 4
                wsi[0] += 1
            else:
                i = slot
            v = wsl[i][:, 0:a * b].rearrange("p (a b) -> p a b", a=a, b=b)
            dma(v, src3, ["scr"], [("w", i)])
            return v, ("w", i)

        bk_rr = {"fm": 0, "rot": 0, "g": 0, "dn": 0}
        ctr = {"xn": 0, "pb": 0, "t": 0, "R": 0, "PT": 0, "onat": 0, "ob": 0}

        def rr(name, n):
            i = ctr[name] % n
            ctr[name] += 1
            return i

        def rmsnorm_T(gi, tl, dstT3, dstkey):
            ssq, ssqk = smcol()
            xi = 0
            S.add("act", act(xn[xi][:], xg3[:, tl, :], AF.Square, accum_out=ssq),
                  [("xg", tl)], [("xn", xi), ssqk])
            _chk("rms_a")
            r, rk = rsqrt_mean(ssq, ssqk, D, RMS_EPS)
            _chk("rms_b")
            S.add("dve", ts(xn[xi][:], xg3[:, tl, :], r, None, ALU.mult), [("xg", tl), rk], [("xn", xi)])
            _chk("rms_c")
            S.add("pe", [tr(ptb3[:, c, :], xn[xi][:, c * 128:(c + 1) * 128]) for c in range(8)],
                  [("xn", xi), "ident"], [PTK])
            _chk("rms_d")
            S.add("act", act(dstT3[:, :, tl * 128:(tl + 1) * 128], ptb3, AF.Copy), [PTK], [(dstkey, tl), "hT_all"])

        for gi in range(NG):
            g0 = gi * 512
            dma(xg3, x_d[g0:g0 + 512, :].rearrange("(t p) d -> p t d", p=128),
                [], [("xg", t_) for t_ in range(4)] + ["xg_all"])
            dma(cfg[:], cf_d[:, g0:g0 + 512], [], ["cfg"])
            dma(sfg[:], sf_d[:, g0:g0 + 512], [], ["sfg"])
            dma(ctT[:], ct_d[:, gi * 128:(gi + 1) * 128], [], ["ctT"])
            dma(stT[:], st_d[:, gi * 128:(gi + 1) * 128], [], ["stT"])
            if gi > 0:
                S.add("pool", cp(bkT[:, 0:128], bkT[:, 512:640]), ["bkT"], ["bkT"])
                S.add("pool", cp(bv[:, 0:130], bv[:, 4 * 130:5 * 130]), ["bv"], ["bv"])
            for tl in range(4):
                rmsnorm_T(gi, tl, hT3, "hT")
            hkeys = [("hT", t_) for t_ in range(4)]
            _chk("rms")

            fm_banks = [0, 1, 4, 5]
            rot_banks = [2, 6]
            fm_w = {}

            def fm_chunk(c0, nk, kind, k, slot):
                if (kind, "w") not in fm_w:
                    fm_w[(kind, "w")] = wload(s_in[:, :, c0:c0 + nk * 128], 8, nk * 128, slot)
                wv, wk = fm_w[(kind, "w")]
                bi = fm_banks[bk_rr["fm"] % 4]
                bk_rr["fm"] += 1
                bo = banks[bi]
                S.add("pe", [mm(bo[:], wv[:, c, k * 128:(k + 1) * 128], hT3[:, c, :], c == 0, c == 7)
                             for c in range(8)], [wk] + hkeys, [B(bi)])
                pi = rr("pb", 2)
                S.add("act", act(pbb[pi][:], bo[:], AF.Copy), [B(bi)], [("pb", pi)])
                ri = rot_banks[bk_rr["rot"] % 2]
                bk_rr["rot"] += 1
                S.add("pe", mm(banks[ri][:], rotT[:], pbb[pi][:], True, True), [("pb", pi), "rotT"], [B(ri)])
                ti = rr("t", 2)
                S.add("dve", tt(T1[ti][:], bo[:], cfg[:], ALU.mult), [B(bi), "cfg"], [("t1", ti)])
                S.add("dve", tt(T2[ti][:], banks[ri][:], sfg[:], ALU.mult), [B(ri), "sfg"], [("t2", ti)])
                addeng = "dve"
                if kind in ("aq", "iq", "bq"):
                    base = {"aq": 0, "iq": 8, "bq": 16}[kind]
                    S.add(addeng, [tt(q3[0:64, base + k, :], T1[ti][0:64, :], T2[ti][0:64, :], ALU.add),
                                   tt(q3[64:128, base + 4 + k, :], T1[ti][64:128, :], T2[ti][64:128, :], ALU.add)],
                          [("t1", ti), ("t2", ti)], [("q", base + k), ("q", base + 4 + k)])
                else:
                    if k == 0:
                        dst, dk = akT[:, g0:g0 + 512], "akT"
                    else:
                        dst, dk = bkT[:, 128:640], "bkT"
                    S.add(addeng, tt(dst, T1[ti][:], T2[ti][:], ALU.add), [("t1", ti), ("t2", ti)], [dk])

            def tm_tile(tl, wv, wk):
                gt = gi * 4 + tl
                bo = banks[3]
                S.add("pe", [mm(bo[:, 0:N_TM], hT3[:, c, tl * 128:(tl + 1) * 128], wv[:, c, :], c == 0, c == 7)
                             for c in range(8)], [wk, ("hT", tl)], [B(3)])
                S.add("act", act(av4[:, gt, :, 0:64], bo[:, 0:128].rearrange("p (g e) -> p g e", g=2), AF.Copy),
                      [B(3)], ["av"])
                S.add("act", act(bv4[:, 1 + tl, :, 0:64], bo[:, 128:256].rearrange("p (g e) -> p g e", g=2), AF.Copy),
                      [B(3)], ["bv"])
                S.add("dve", ts(iw3[:, tl, :], bo[:, 320:328], W_SCALE, None, ALU.mult), [B(3)], [("iw", tl)])
                s1, s1k = smcol()
                S.add("dve", ts(ikx[:], bo[:, 256:320], 1.0, 0.0, ALU.mult, ALU.add, accum_out=s1),
                      [B(3)], ["ikx", s1k])
                nm, nmk = smcol()
                S.add("dve", ts(nm, s1, -1.0 / 64, None, ALU.mult), [s1k], [nmk])
                S.add("dve", ts(ikc[:], ikx[:], nm, None, ALU.add), ["ikx", nmk], ["ikc"])
                s2, s2k = smcol()
                S.add("act", act(ikx[:], ikc[:], AF.Square, accum_out=s2), ["ikc", "ikx"], ["ikx", s2k])
                r, rk = rsqrt_mean(s2, s2k, 64, LN_EPS)
                S.add("dve", stt(ikc[:], ikc[:], r, iknw[:], ALU.mult, ALU.mult), ["ikc", rk, "iknw"], ["ikc"])
                S.add("dve", tt(ikc[:], ikc[:], iknb[:], ALU.add), ["ikc", "iknb"], ["ikc"])
                c_ = ct3[:, tl, :]
                s_ = st3[:, tl, :]
                S.add("dve", [tt(ikx[:, 0:32], ikc[:, 0:32], c_, ALU.mult),
                              tt(ikx[:, 32:64], ikc[:, 32:64], c_, ALU.mult)], ["ikc", "ctT", "ikx"], ["ikx"])
                S.add("dve", [tt(dtmp[:, 0:32], ikc[:, 32:64], s_, ALU.mult),
                              tt(dtmp[:, 32:64], ikc[:, 0:32], s_, ALU.mult)], ["ikc", "stT"], ["dtmp"])
                S.add("dve", [tt(ikr[:, 0:32], ikx[:, 0:32], dtmp[:, 0:32], ALU.subtract),
                              tt(ikr[:, 32:64], ikx[:, 32:64], dtmp[:, 32:64], ALU.add)],
                      ["ikx", "dtmp"], ["ikr"])
                S.add("dve", cp(ikr[:, 64:128], ikr[:, 0:64]), ["ikr"], ["ikr"])
                S.add("pe", tr(ptb[:, 0:128], ikr[:]), ["ikr", "ident"], [PTK])
                S.add("act", act(ikT[:, gt * 128:(gt + 1) * 128], ptb[:, 0:128], AF.Copy), [PTK], ["ikT"])

            tmw = wload(s_in[:, :, C_TM:C_TM + N_TM], 8, N_TM, 3)
            chunks = ([(C_IQ, 4, "iq", k, 1) for k in range(4)] + [(C_AK, 2, "kk", k, 0) for k in range(2)]
                      + [(C_AQ, 4, "aq", k, 2) for k in range(4)] + [(C_BQ, 4, "bq", k, 1) for k in range(4)])
            per = [4, 4, 3, 3]
            ci = 0
            for tl in range(4):
                tm_tile(tl, *tmw)
                for _ in range(per[tl]):
                    fm_chunk(*chunks[ci])
                    ci += 1

            if gi == 0:
                dump("akT", akT[:, 0:512], [128, 512], BF16, "akT")
                dump("ikT", ikT[:, 0:512], [128, 512], BF16, "ikT")
                dump("av", av[:, 0:520], [128, 520], BF16, "av")
                dump("iw", iw[:], [128, 32], F32, [("iw", t_) for t_ in range(4)])

            _chk("stage1")
            def idx_phase(tl):
                gt = gi * 4 + tl
                nk = (gt + 1) * 128
                diag3 = diag3s[tl % 2]
                dgk = ("diag", tl % 2)
                sc_ = scs[tl % 2]
                sck = ("scores", tl % 2)
                S.add("pool", [ts(diag3[:, h, :], ident[:], iw3[:, tl, h:h + 1], None, ALU.mult) for h in range(8)],
                      [("iw", tl), "ident"], [dgk])
                nchunk = (nk + 511) // 512
                for c in range(nchunk):
                    c0 = c * 512
                    ncol = min(512, nk - c0)
                    last = (c == nchunk - 1)
                    pend = []

                    def dmm(hh, rj):
                        fns = [mm(banks[2][:, 0:ncol], diag3[:, hh, :], Rb[rj][:, 0:ncol], hh == 0, (hh == 7) and not last)]
                        rd_ = [("R", rj), dgk]
                        if hh == 7 and last:
                            fns.append(mm(banks[2][:, ncol - 128:ncol], ident[:], cmask[:], False, True))
                            rd_ += ["ident", "cmask"]
                        S.add("pe", fns, rd_, [B(2)])

                    for h in range(8):
                        half, kq = h % 2, h // 2
                        sbk = h % 2
                        S.add("pe", mm(banks[sbk][:, 0:ncol],
                                       q3[:, 8 + half * 4 + kq, tl * 128:(tl + 1) * 128],
                                       ikT[:, c0:c0 + ncol], True, True),
                              [("q", 8 + half * 4 + kq), "ikT"], [B(sbk)])
                        ri = rr("R", 3)
                        S.add("act", act(Rb[ri][:, 0:ncol], banks[sbk][:, 0:ncol], AF.Relu), [B(sbk)], [("R", ri)])
                        pend.append((h, ri))
                        if len(pend) == 2:
                            dmm(*pend.pop(0))
                    while pend:
                        dmm(*pend.pop(0))
                    S.add("act", act(sc_[:, c0:c0 + ncol], banks[2][:, 0:ncol], AF.Copy), [B(2)], [sck])
                return nk

            def thr_phase(tl):
                gt = gi * 4 + tl
                nk = (gt + 1) * 128
                thr, thrk = smcol()
                sc_ = scs[tl % 2]
                sck = ("scores", tl % 2)
                if gt < 2:
                    S.add("dve", lambda e: e.memset(thr, -1.0e29), [], [thrk])
                else:
                    nd = nk - 128
                    aa, aak = smcol()
                    dmx, dmxk = smcol()
                    hi, hik = smcol()
                    S.add("dve", lambda e: e.tensor_reduce(aa, sc_[:, 0:nd], AX.X, ALU.max, apply_absolute_value=True),
                          [sck], [aak])
                    S.add("dve", lambda e: e.tensor_reduce(dmx, sc_[:, nd:nk], AX.X, ALU.max), [sck], [dmxk])
                    S.add("dve", tt(hi, aa, dmx, ALU.max), [aak, dmxk], [hik])
                    w0, w0k = smcol()
                    S.add("dve", stt(w0, hi, 1.0, aa, ALU.add, ALU.add), [hik, aak], [w0k])
                    S.add("dve", ts(hwk[:], pow2[:], w0, None, ALU.mult), ["pow2", w0k], ["hwk"])
                    mid, midk = smcol()
                    S.add("dve", tt(mid, hwk[:, 0:1], aa, ALU.subtract), [aak, "hwk"], [midk])
                    cnt, cntk = smcol()
                    stp, stpk = smcol()
                    for it in range(NIT):
                        S.add("dve", ts(junk[:, 0:nk], sc_[:, 0:nk], mid, 0.0, ALU.is_ge, ALU.add, accum_out=cnt),
                              [sck, midk], [JKK, cntk])
                        S.add("dve", ts(stp, cnt, float(TOPK), hwk[:, it:it + 1], ALU.is_ge, ALU.mult),
                              [cntk, "hwk"], [stpk])
                        S.add("dve", stt(mid, stp, hwk[:, it + 1:it + 2], mid, ALU.subtract, ALU.add),
                              [stpk, "hwk", midk], [midk])
                    S.add("dve", tt(thr, mid, hwk[:, NIT:NIT + 1], ALU.subtract), [midk, "hwk"], [thrk])
                S.add("dve", ts(mbs[tl % 2][:, 0:nk], sc_[:, 0:nk], thr, NEG, ALU.is_lt, ALU.mult),
                      [sck, thrk], [("mb", tl % 2)])

            def attn_core(tl, blocks, qbase, kfun, vfun, biasfun, sink, ochunk0):
                first = [True, True]
                pend = []

                def pv(j, g, pi):
                    vap, vk = vfun(j, g)
                    fns = []
                    for hh in range(4):
                        fns.append(mm(banks[5 + g][:, hh * 65:(hh + 1) * 65], PTb[pi][:, hh * 128:(hh + 1) * 128],
                                      vap, first[g] and hh == 0, False))
                    first[g] = False
                    S.add("pe", fns, [("PT", pi), vk], [B(5 + g)])

                stb = [3, 4]
                n = 0
                for j in blocks:
                    for g in range(2):
                        kap, kk = kfun(j, g)
                        bl, br, bkeys = biasfun(j)
                        sbi = stb[n % 2]
                        n += 1
                        S.add("pe", [mm(banks[sbi][:], kap, q3[:, qbase + g * 4:qbase + g * 4 + 4,
                                                                tl * 128:(tl + 1) * 128], True, False),
                                     mm(banks[sbi][:], bl, br, False, True)],
                              [kk] + [("q", qbase + g * 4 + k) for k in range(4)] + bkeys, [B(sbi)])
                        pi = rr("PT", 3)
                        S.add("act", act(PTb[pi][:], banks[sbi][:], AF.Exp, scale=0.125), [B(sbi)], [("PT", pi)])
                        pend.append((j, g, pi))
                        if len(pend) == 2:
                            pv(*pend.pop(0))
                while pend:
                    pv(*pend.pop(0))
                oi = rr("onat", 3)
                for g in range(2):
                    o3 = banks[5 + g][:, 0:260].rearrange("p (h e) -> p h e", e=65)
                    if sink:
                        S.add("dve", tt(rd[:, g * 4:(g + 1) * 4], o3[:, :, 64], esink[:, g * 4:(g + 1) * 4], ALU.add),
                              [B(5 + g), "esink"], [("rd", g)])
                        S.add("dve", lambda e, g=g: e.reciprocal(rd[:, g * 4:(g + 1) * 4], rd[:, g * 4:(g + 1) * 4]),
                              [("rd", g)], [("rd", g)])
                    else:
                        S.add("dve", lambda e, g=g, o3=o3: e.reciprocal(rd[:, g * 4:(g + 1) * 4], o3[:, :, 64]),
                              [B(5 + g)], [("rd", g)])
                    S.add("dve", [ts(onat[oi][:, (g * 4 + hh) * 64:(g * 4 + hh + 1) * 64], o3[:, hh, 0:64],
                                     rd[:, g * 4 + hh:g * 4 + hh + 1], None, ALU.mult) for hh in range(4)],
                          [B(5 + g), ("rd", g)], [("onat", oi)])
                def finish():
                    S.add("pe", [tr(ptb3[:, c, :], onat[oi][:, c * 128:(c + 1) * 128]) for c in range(4)],
                          [("onat", oi), "ident"], [PTK])
                    S.add("act", act(oT3[:, ochunk0:ochunk0 + 4, tl * 128:(tl + 1) * 128], ptb3[:, 0:4, :], AF.Copy),
                          [PTK], [("oT", tl), "oT_all"])
                return finish

            def swa_phase(tl):
                gt = gi * 4 + tl
                blocks = ([0] if gt > 0 else []) + [1]

                def kfun(j, g):
                    c0 = (tl + j) * 128
                    return bkT[:, c0:c0 + 128], "bkT"

                def vfun(j, g):
                    return bv4[:, tl + j, g, :], "bv"

                def biasfun(j):
                    return ident[:], (swap[:] if j == 0 else swac[:]), ["ident", "swap", "swac"]

                return attn_core(tl, blocks, 16, kfun, vfun, biasfun, True, 4)

            def dsa_phase(tl):
                gt = gi * 4 + tl

                def kfun(j, g):
                    return akT[:, j * 128:(j + 1) * 128], "akT"

                def vfun(j, g):
                    return av4[:, j, g, :], "av"

                def biasfun(j):
                    return mbs[tl % 2][:, j * 128:(j + 1) * 128], ident4[:], [("mb", tl % 2), "ident4"]

                return attn_core(tl, list(range(gt + 1)), 0, kfun, vfun, biasfun, False, 0)

            idx_phase(0)
            _chk("idx0")
            thr_phase(0)
            _chk("thr0")
            pending_fin = None
            for tl in range(4):
                if tl < 3:
                    idx_phase(tl + 1)
                fin_swa = swa_phase(tl)
                fin_swa()
                if pending_fin is not None:
                    pending_fin()
                    pending_fin = None
                if tl < 3:
                    thr_phase(tl + 1)
                if gi == 0 and tl == 2:
                    dump("scores", scs[1][:, 0:512], [128, 512], F32, ("scores", 1))
                    dump("mb", mbs[1][:, 0:512], [128, 512], BF16, ("mb", 1))
                pending_fin = dsa_phase(tl)
            pending_fin()
            if gi == 0:
                dump("oT", oT[:], [128, 4096], BF16, [("oT", t_) for t_ in range(4)])

            _chk("stage2")
            okeys = [("oT", t_) for t_ in range(4)]
            g_banks = [0, 1, 2, 3, 4, 5, 6]
            for half in range(2):
                if half == 0:
                    wba_v, wba_k = wload(s_ba, 4, D, 1)
                    wbb_v, wbb_k = wload(s_bb, 4, D, 2)
                wga_v, wga_k = wload(s_in[:, :, C_GA + half * 512:C_GA + (half + 1) * 512], 8, 512, 3)
                wgb_v, wgb_k = wload(s_in[:, :, C_GB + half * 512:C_GB + (half + 1) * 512], 8, 512, 0)
                for cc in range(4):
                    c = half * 4 + cc
                    bs = []
                    for _ in range(4):
                        bs.append(g_banks[bk_rr["g"] % 7])
                        bk_rr["g"] += 1
                    bya, byb, bga, bgb = bs
                    S.add("pe", [mm(banks[bya][:], wba_v[:, e_, c * 128:(c + 1) * 128], oT3[:, e_, :], e_ == 0, e_ == 3)
                                 for e_ in range(4)], [wba_k] + okeys, [B(bya)])
                    S.add("pe", [mm(banks[byb][:], wbb_v[:, e_, c * 128:(c + 1) * 128], oT3[:, 4 + e_, :], e_ == 0, e_ == 3)
                                 for e_ in range(4)], [wbb_k] + okeys, [B(byb)])
                    S.add("pe", [mm(banks[bga][:], wga_v[:, d_, cc * 128:(cc + 1) * 128], hT3[:, d_, :], d_ == 0, d_ == 7)
                                 for d_ in range(8)], [wga_k] + hkeys, [B(bga)])
                    S.add("pe", [mm(banks[bgb][:], wgb_v[:, d_, cc * 128:(cc + 1) * 128], hT3[:, d_, :], d_ == 0, d_ == 7)
                                 for d_ in range(8)], [wgb_k] + hkeys, [B(bgb)])
                    ti = rr("t", 2)
                    S.add("act", act(T1[ti][:], banks[bga][:], AF.Tanh, scale=0.5), [B(bga)], [("t1", ti)])
                    S.add("act", act(T2[ti][:], banks[bgb][:], AF.Tanh, scale=0.5), [B(bgb)], [("t2", ti)])
                    S.add("dve", stt(T1[ti][:], T1[ti][:], 1.0, banks[bya][:], ALU.add, ALU.mult),
                          [("t1", ti), B(bya)], [("t1", ti)])
                    S.add("dve", stt(T2[ti][:], T2[ti][:], 1.0, banks[byb][:], ALU.add, ALU.mult),
                          [("t2", ti), B(byb)], [("t2", ti)])
                    S.add("pool", tt(mixT3[:, c, :], T1[ti][:], T2[ti][:], ALU.add),
                          [("t1", ti), ("t2", ti)], [("mix", c), ("scores", 1)])
            mkeys = [("mix", c) for c in range(8)] + [("scores", 1)]
            for half in range(2):
                wo_v, wo_k = wload(s_out[:, :, half * 512:(half + 1) * 512], 8, 512, 1 + half)
                for tl in range(4):
                    bi = g_banks[bk_rr["g"] % 7]
                    bk_rr["g"] += 1
                    S.add("pe", [mm(banks[bi][:], mixT3[:, d_, tl * 128:(tl + 1) * 128], wo_v[:, d_, :], d_ == 0, d_ == 7)
                                 for d_ in range(8)], [wo_k] + mkeys, [B(bi)])
                    S.add("dve", tt(xg3[:, tl, half * 512:(half + 1) * 512], xg3[:, tl, half * 512:(half + 1) * 512],
                                    banks[bi][:], ALU.add), [B(bi), ("xg", tl)], [("xg", tl)])
            if gi == 0:
                dump("x1", xg[:], [128, 4096], F32, [("xg", t_) for t_ in range(4)])

            _chk("stage3")
            for tl in range(4):
                rmsnorm_T(gi, tl, hT3, "hT")
            def mlp_up(hc):
                wu_v, wu_k = wload(s_up[:, :, hc * 512:(hc + 1) * 512], 8, 512, [3, 1][hc % 2])
                wd = wload(s_dn[:, hc * 4:(hc + 1) * 4, :], 4, D, [0, 2][hc % 2])
                u3 = u2Ts[hc % 2]
                for fc in range(4):
                    bi = [0, 1][fc % 2]
                    S.add("pe", [mm(banks[bi][:], wu_v[:, d_, fc * 128:(fc + 1) * 128], hT3[:, d_, :], d_ == 0, d_ == 7)
                                 for d_ in range(8)], [wu_k] + hkeys, [B(bi)])
                    ti = rr("t", 2)
                    S.add("act", act(T1[ti][:], banks[bi][:], AF.Relu), [B(bi)], [("t1", ti)])
                    S.add("pool", tt(u3[:, fc, :], T1[ti][:], T1[ti][:], ALU.mult), [("t1", ti)], [("u2", 0, fc)])
                return wd

            def mlp_down(hc, wd):
                wd_v, wd_k = wd
                u3 = u2Ts[hc % 2]
                ukeys = [("u2", 0, fc) for fc in range(4)]
                for tl in range(4):
                    for half in range(2):
                        bi = [2, 3, 4, 5, 6][bk_rr["dn"] % 5]
                        bk_rr["dn"] += 1
                        S.add("pe", [mm(banks[bi][:], u3[:, fc, tl * 128:(tl + 1) * 128],
                                        wd_v[:, fc, half * 512:(half + 1) * 512], fc == 0, fc == 3) for fc in range(4)],
                              [wd_k] + ukeys, [B(bi)])
                        S.add("dve", tt(xg3[:, tl, half * 512:(half + 1) * 512], xg3[:, tl, half * 512:(half + 1) * 512],
                                        banks[bi][:], ALU.add), [B(bi), ("xg", tl)], [("xg", tl)])

            for hc in range(8):
                mlp_down(hc, mlp_up(hc))
            for tl in range(4):
                ssq, ssqk = smcol()
                oi = 0
                S.add("act", act(obuf[oi], xg3[:, tl, :], AF.Square, accum_out=ssq), [("xg", tl)], [OBK, ssqk])
                r, rk = rsqrt_mean(ssq, ssqk, D, RMS_EPS)
                S.add("dve", stt(obuf[oi], xg3[:, tl, :], r, fnw[:], ALU.mult, ALU.mult),
                      [("xg", tl), rk, "fnw"], [OBK])
                r0 = g0 + tl * 128
                dma(out_d[r0:r0 + 128, :], obuf[oi], [OBK], [])

    except _Stop:
        pass

    def emit(name, eng):
        for fns, waits, me, is_dma in S.ops[name]:
            for k, v in waits:
                eng.wait_ge(sems[k], v)
            ins = None
            for f in fns:
                ins = f(eng)
            ins.then_inc(sems[me[0]], 16 if is_dma else 1)
        if name == "sp":
            for i in range(NDMA):
                if S.dma_val[i] > 0:
                    eng.wait_ge(sems[("dma", i)], S.dma_val[i])

    with nc.Block() as block:
        @block.sync
        def _(e):
            emit("sp", e)

        @block.tensor
        def _(e):
            emit("pe", e)

        @block.scalar
        def _(e):
            emit("act", e)

        @block.vector
        def _(e):
            emit("dve", e)

        @block.gpsimd
        def _(e):
            emit("pool", e)
    print("SBUF bytes remaining:", nc.sbuf_bytes_remaining, "ops:", {k: len(v) for k, v in S.ops.items()})
    es.close()
    return nc, dbg_d


def _bf(a):
    return np.asarray(a, dtype=np.float32).astype(ml_dtypes.bfloat16)


def host_consts(L):
    NT = L // 128
    inv = (1.0 / (np.float32(10000.0) ** (np.arange(0, 64, 2, dtype=np.float32) / np.float32(64)))).astype(np.float32)
    ang = (np.arange(L, dtype=np.float32)[:, None] * inv[None, :]).astype(np.float32)
    cos, sin = np.cos(ang).astype(np.float32), np.sin(ang).astype(np.float32)
    p = np.arange(128)
    cf = np.ascontiguousarray(cos[:, p % 32].T)
    sf = np.ascontiguousarray(sin[:, p % 32].T)
    ct = np.ascontiguousarray(cos.reshape(NT, 128, 32).transpose(1, 0, 2).reshape(128, NT * 32))
    st = np.ascontiguousarray(sin.reshape(NT, 128, 32).transpose(1, 0, 2).reshape(128, NT * 32))
    ident = np.eye(128, dtype=np.float32)
    rot = np.zeros((128, 128), np.float32)
    for m in range(128):
        if (m % 64) < 32:
            rot[m + 32, m] = -1.0
        else:
            rot[m - 32, m] = 1.0
    sp = np.arange(128)[:, None]
    tp = np.arange(128)[None, :]
    cur = np.where(sp <= tp, 0.0, NEG).astype(np.float32)
    prev = np.where(sp > tp, 0.0, NEG).astype(np.float32)
    cmask = np.where(np.arange(128)[None, :] <= np.arange(128)[:, None], 0.0, SNEG).astype(np.float32)
    pow2 = np.tile((0.5 ** np.arange(1, NIT + 2, dtype=np.float64)).astype(np.float32)[None, :], (128, 1))
    return {
        "ropeCf": cf, "ropeSf": sf, "ropeCt": ct, "ropeSt": st,
        "ident": _bf(ident), "ident4": _bf(np.tile(ident, (1, 4))), "rotT": _bf(rot),
        "swa_cur": _bf(np.tile(cur, (1, 4))), "swa_prev": _bf(np.tile(prev, (1, 4))),
        "cmask": _bf(cmask), "pow2": pow2,
    }


def permute_w_in(w):
    a_q, a_k, a_v = w[:, 0:512], w[:, 512:640], w[:, 640:768]
    i_q, i_k, i_w = w[:, 768:1280], w[:, 1280:1344], w[:, 1344:1352]
    b_q, b_k, b_v = w[:, 1352:1864], w[:, 1864:1992], w[:, 1992:2120]
    g_a, g_b = w[:, 2120:3144], w[:, 3144:4168]

    def grp(q):
        cols = []
        for k in range(4):
            cols.append(q[:, k * 64:(k + 1) * 64])
            cols.append(q[:, (4 + k) * 64:(5 + k) * 64])
        return np.concatenate(cols, axis=1)

    return np.ascontiguousarray(np.concatenate(
        [grp(a_q), i_q, grp(b_q), a_k, b_k, g_a, g_b, a_v, b_v, i_k, i_w], axis=1))


_CACHE = {}


def make_in_maps(L, x, attn_norm_w, w_in, idx_k_norm_w, idx_k_norm_b, sinks, w_branch_a, w_branch_b,
                 w_out, mlp_norm_w, w_up, w_down, final_norm_w):
    f = lambda a: np.ascontiguousarray(np.asarray(a, dtype=np.float32))
    shared = dict(host_consts(L))
    shared.update({
        "w_in": permute_w_in(f(w_in[0])),
        "w_ba": f(w_branch_a[0]), "w_bb": f(w_branch_b[0]), "w_out": f(w_out[0]),
        "w_up": f(w_up[0]), "w_dn": f(w_down[0]),
        "anw": f(np.asarray(attn_norm_w[0]).reshape(8, 128).T),
        "mnw": f(np.asarray(mlp_norm_w[0]).reshape(8, 128).T),
        "fnw": f(np.tile(np.asarray(final_norm_w).reshape(1, D), (128, 1))),
        "iknw": f(np.tile(np.asarray(idx_k_norm_w[0]).reshape(1, 64), (128, 1))),
        "iknb": f(np.tile(np.asarray(idx_k_norm_b[0]).reshape(1, 64), (128, 1))),
        "sinks": f(np.tile(np.asarray(sinks[0]).reshape(1, 8), (128, 1))),
    })
    x = np.asarray(x, dtype=np.float32)
    maps = []
    for b in range(x.shape[0]):
        m = dict(shared)
        m["x"] = np.ascontiguousarray(x[b, :L])
        maps.append(m)
    return maps


def run(L, inputs, dbg=(), n_cores=8, trace=False):
    key = (L, tuple(dbg))
    nc, dbg_d = build_program(L, dbg)
    maps = make_in_maps(L, **inputs)[:n_cores]
    res = run_bass_kernel_spmd(nc, maps, core_ids=list(range(len(maps))), trace=trace)
    return res


def kernel(**inputs):
    L = 4096
    res = run(L, inputs)
    out = np.stack([np.asarray(r["out"], dtype=np.float32) for r in res.results], axis=0)
    return out
```
